# Optimizing a Trainium2 kernel written in Bass

```python
import math
import jax, jax.numpy as jnp
from jax import lax
import numpy as np

D_MODEL = 1024
BATCH = 8
SEQ = 2048
DEPTH = 2

GRID_W = 64
CTX_LEN = 256
N_MIXERS = 2
N_ATTN_LAYERS = (DEPTH + N_MIXERS - 1) // N_MIXERS
N_HGRN_LAYERS = DEPTH // N_MIXERS
DA_HEADS = 8
DA_QK_DIM = D_MODEL // (2 * DA_HEADS)
DA_V_DIM = 2 * DA_QK_DIM
ROPE_BASE = 10000.0
Q_BLOCK = 128
HG_HEADS = 8
HG_DIM = D_MODEL // HG_HEADS
CHUNK = 64
N_EXPERTS = 16
N_GROUPS = 4
EXPERTS_PER_GROUP = N_EXPERTS // N_GROUPS
TOP_K = 2
D_FF = D_MODEL // 2
EPS = 1e-6

kernel_name = "hybrid_diffattn_hgrn2_groupmoe_dit"


def rms_norm(x, g):
    xf = x.astype(jnp.float32)
    y = xf * lax.rsqrt(jnp.mean(xf * xf, axis=-1, keepdims=True) + EPS)
    return (y * g.astype(jnp.float32)).astype(x.dtype)


def axial_rope_tables(n_tokens):
    rows = n_tokens // GRID_W
    r, col = jnp.meshgrid(jnp.arange(rows), jnp.arange(GRID_W), indexing="ij")
    r = r.reshape(-1).astype(jnp.float32)
    col = col.reshape(-1).astype(jnp.float32)
    n_pairs = DA_QK_DIM // 4
    inv = ROPE_BASE ** (-jnp.arange(n_pairs, dtype=jnp.float32) / n_pairs)
    ang = jnp.concatenate([r[:, None] * inv, col[:, None] * inv], axis=-1)
    return jnp.cos(ang), jnp.sin(ang)


def apply_rope(x, cos, sin):
    xf = x.astype(jnp.float32)
    half = xf.shape[-1] // 2
    x1, x2 = xf[..., :half], xf[..., half:]
    cs, sn = cos[None, :, None, None, :], sin[None, :, None, None, :]
    return jnp.concatenate([x1 * cs - x2 * sn, x2 * cs + x1 * sn], axis=-1).astype(x.dtype)


def diff_attention_mixer(h, hc, w_qkv, w_o, q_norm, k_norm, sub_norm, lam_p, layer_idx, need_ctx_out):
    B, T, _ = h.shape
    lam_init = 0.8 - 0.6 * math.exp(-0.3 * layer_idx)
    lp = lam_p.astype(jnp.float32)
    lam = jnp.exp(jnp.sum(lp[0] * lp[1])) - jnp.exp(jnp.sum(lp[2] * lp[3])) + lam_init
    scale = 1.0 / math.sqrt(DA_QK_DIM)

    def project(t):
        b_, t_, _ = t.shape
        q, k, v = jnp.split(t @ w_qkv, 3, axis=-1)
        q = rms_norm(q.reshape(b_, t_, DA_HEADS, 2, DA_QK_DIM), q_norm)
        k = rms_norm(k.reshape(b_, t_, DA_HEADS, 2, DA_QK_DIM), k_norm)
        return q, k, v.reshape(b_, t_, DA_HEADS, DA_V_DIM)

    def attend(qb, keys, vals):
        s = jnp.einsum("bqhmd,bkhmd->bhmqk", qb.astype(jnp.float32), keys.astype(jnp.float32)) * scale
        p = jax.nn.softmax(s, axis=-1)
        a = p[:, :, 0] - lam * p[:, :, 1]
        return jnp.einsum("bhqk,bkhe->bqhe", a, vals.astype(jnp.float32))

    def head_out(o):
        o = rms_norm(o, sub_norm) * (1.0 - lam_init)
        return o.reshape(o.shape[0], o.shape[1], DA_HEADS * DA_V_DIM).astype(h.dtype) @ w_o

    q, k, v = project(h)
    cos, sin = axial_rope_tables(T)
    q, k = apply_rope(q, cos, sin), apply_rope(k, cos, sin)
    qc, kc, vc = project(hc)

    k_all = jnp.concatenate([k, kc], axis=1)
    v_all = jnp.concatenate([v, vc], axis=1)
    nb = T // Q_BLOCK
    q_blocks = q.reshape(B, nb, Q_BLOCK, DA_HEADS, 2, DA_QK_DIM).swapaxes(0, 1)
    o = lax.map(lambda qb: attend(qb, k_all, v_all), q_blocks)
    out = head_out(o.swapaxes(0, 1).reshape(B, T, DA_HEADS, DA_V_DIM))
    out_c = head_out(attend(qc, kc, vc)) if need_ctx_out else None
    return out, out_c


def gla_chunk_scan(q, k, v, logf, s0):
    B, T, H, K = q.shape
    V = v.shape[-1]
    n = T // CHUNK

    def to_chunks(t):
        return t.reshape(B, n, CHUNK, H, t.shape[-1]).swapaxes(0, 1)

    mask = jnp.tril(jnp.ones((CHUNK, CHUNK), dtype=bool))[None, :, :, None, None]

    def step(S, xs):
        qc, kc, vc, lf = xs
        b = jnp.cumsum(lf, axis=1)
        o_inter = jnp.einsum("bchk,bhkv->bchv", qc * jnp.exp(b), S)
        rel = b[:, :, None] - b[:, None, :]
        decay = jnp.where(mask, jnp.exp(jnp.where(mask, rel, 0.0)), 0.0)
        scores = jnp.einsum("bthk,bshk,btshk->bhts", qc, kc, decay)
        o_intra = jnp.einsum("bhts,bshv->bthv", scores, vc)
        b_last = b[:, -1]
        S_new = jnp.exp(b_last)[..., None] * S + jnp.einsum(
            "bshk,bshv->bhkv", kc * jnp.exp(b_last[:, None] - b), vc)
        return S_new, o_inter + o_intra

    S_fin, o = lax.scan(step, s0, (to_chunks(q), to_chunks(k), to_chunks(v), to_chunks(logf)))
    return o.swapaxes(0, 1).reshape(B, T, H, V), S_fin


def hgrn2_mixer(h, hc, w_in, w_o, out_norm, lb, need_ctx_out):
    lb = lb.astype(jnp.float32)

    def project(t):
        b_, t_, _ = t.shape
        q, v, zf, zb, g = jnp.split(t @ w_in, 5, axis=-1)
        shp = (b_, t_, HG_HEADS, HG_DIM)
        q = jax.nn.silu(q.astype(jnp.float32)).reshape(shp)
        v = v.astype(jnp.float32).reshape(shp)
        zf = zf.astype(jnp.float32).reshape(shp)
        zb = zb.astype(jnp.float32).reshape(shp)
        return q, v, zf, zb, g.astype(jnp.float32).reshape(shp)

    def forget(z, lbd):
        logf = jnp.logaddexp(jnp.log(lbd), jnp.log1p(-lbd) + jax.nn.log_sigmoid(z))
        return (1.0 - lbd) * jax.nn.sigmoid(-z), logf

    def flip(t):
        return jnp.flip(t, axis=1)

    def bidir(q, v, zf, zb, s_f, s_b):
        kf, lff = forget(zf, lb[0])
        kb, lfb = forget(zb, lb[1])
        o_f, S_f = gla_chunk_scan(q, kf, v, lff, s_f)
        o_b, S_b = gla_chunk_scan(flip(q), flip(kb), flip(v), flip(lfb), s_b)
        return o_f + flip(o_b), S_f, S_b

    def readout(o, g):
        o = rms_norm(o, out_norm) * jax.nn.silu(g)
        return o.reshape(o.shape[0], o.shape[1], HG_HEADS * HG_DIM).astype(h.dtype) @ w_o

    qc, vc, zfc, zbc, gc = project(hc)
    s0 = jnp.zeros((hc.shape[0], HG_HEADS, HG_DIM, HG_DIM), jnp.float32)
    oc, Sc_f, Sc_b = bidir(qc, vc, zfc, zbc, s0, s0)
    q, v, zf, zb, g = project(h)
    o, _, _ = bidir(q, v, zf, zb, Sc_f, Sc_b)
    out_c = readout(oc, gc) if need_ctx_out else None
    return readout(o, g), out_c


def moe_ffn(t, router_w, router_bias, w_gate, w_up, w_down):
    n = t.shape[0]
    aff = jax.nn.sigmoid(t.astype(jnp.float32) @ router_w.astype(jnp.float32))
    biased = aff + router_bias.astype(jnp.float32)
    grp_score = lax.top_k(biased.reshape(n, N_GROUPS, EXPERTS_PER_GROUP), TOP_K)[0].sum(-1)
    sel_group = jnp.argmax(grp_score, axis=-1)
    in_group = (jnp.arange(N_EXPERTS) // EXPERTS_PER_GROUP)[None, :] == sel_group[:, None]
    _, idx = lax.top_k(jnp.where(in_group, biased, -jnp.inf), TOP_K)
    w = jnp.take_along_axis(aff, idx, axis=-1)
    w = w / jnp.sum(w, axis=-1, keepdims=True)
    gates = jnp.sum(jax.nn.one_hot(idx, N_EXPERTS, dtype=jnp.float32) * w[..., None], axis=1)
    y = jnp.zeros((n, t.shape[1]), jnp.float32)
    for e in range(N_EXPERTS):
        hid = jax.nn.silu(t @ w_gate[e]) * (t @ w_up[e])
        y = y + gates[:, e:e + 1] * (hid @ w_down[e]).astype(jnp.float32)
    return y.astype(t.dtype)


def setup_inputs(seed: int = 0) -> dict:
    key = jax.random.key(seed)
    ks = jax.random.split(key, 24)
    D, HK = D_MODEL, HG_HEADS * HG_DIM
    nrm = jax.random.normal
    s = D ** -0.5
    return {
        "x": nrm(ks[0], (BATCH, SEQ, D), jnp.float32),
        "c": nrm(ks[1], (BATCH, D), jnp.float32),
        "ctx": nrm(ks[2], (BATCH, CTX_LEN, D), jnp.float32),
        "c_ctx": nrm(ks[3], (D,), jnp.float32),
        "ada_w": nrm(ks[4], (DEPTH, D, 6 * D), jnp.float32) * (0.5 * s),
        "ada_b": nrm(ks[5], (DEPTH, 6 * D), jnp.float32) * 0.02,
        "norm_mix": 1.0 + 0.1 * nrm(ks[6], (DEPTH, D), jnp.float32),
        "norm_ffn": 1.0 + 0.1 * nrm(ks[7], (DEPTH, D), jnp.float32),
        "attn_w_qkv": nrm(ks[8], (N_ATTN_LAYERS, D, 3 * D), jnp.float32) * s,
        "attn_w_o": nrm(ks[9], (N_ATTN_LAYERS, DA_HEADS * DA_V_DIM, D), jnp.float32) * s,
        "attn_q_norm": 1.0 + 0.1 * nrm(ks[10], (N_ATTN_LAYERS, DA_QK_DIM), jnp.float32),
        "attn_k_norm": 1.0 + 0.1 * nrm(ks[11], (N_ATTN_LAYERS, DA_QK_DIM), jnp.float32),
        "attn_sub_norm": 1.0 + 0.1 * nrm(ks[12], (N_ATTN_LAYERS, DA_V_DIM), jnp.float32),
        "attn_lambda": 0.1 * nrm(ks[13], (N_ATTN_LAYERS, 4, DA_QK_DIM), jnp.float32),
        "hgrn_w_in": nrm(ks[14], (N_HGRN_LAYERS, D, 5 * HK), jnp.float32) * s,
        "hgrn_w_o": nrm(ks[15], (N_HGRN_LAYERS, HK, D), jnp.float32) * s,
        "hgrn_out_norm": 1.0 + 0.1 * nrm(ks[16], (N_HGRN_LAYERS, HG_DIM), jnp.float32),
        "hgrn_lb_gamma": nrm(ks[17], (2, DEPTH, HK), jnp.float32),
        "router_w": nrm(ks[18], (D, N_EXPERTS), jnp.float32) * s,
        "router_bias": 0.01 * nrm(ks[19], (N_EXPERTS,), jnp.float32),
        "moe_w_gate": nrm(ks[20], (DEPTH, N_EXPERTS, D, D_FF), jnp.float32) * s,
        "moe_w_up": nrm(ks[21], (DEPTH, N_EXPERTS, D, D_FF), jnp.float32) * s,
        "moe_w_down": nrm(ks[22], (DEPTH, N_EXPERTS, D_FF, D), jnp.float32) * (D_FF ** -0.5),
    }


def reference(x, c, ctx, c_ctx, ada_w, ada_b, norm_mix, norm_ffn, attn_w_qkv, attn_w_o,
              attn_q_norm, attn_k_norm, attn_sub_norm, attn_lambda, hgrn_w_in, hgrn_w_o,
              hgrn_out_norm, hgrn_lb_gamma, router_w, router_bias, moe_w_gate, moe_w_up,
              moe_w_down):
    B, T, D = x.shape
    P = jax.nn.softmax(hgrn_lb_gamma.astype(jnp.float32), axis=1)
    cum = jnp.cumsum(P, axis=1)
    lb_all = (cum - cum[:, :1]).reshape(2, DEPTH, HG_HEADS, HG_DIM)

    xc = ctx
    for i in range(DEPTH):
        need_ctx = i < DEPTH - 1
        mod = jax.nn.silu(c) @ ada_w[i] + ada_b[i]
        sh_m, sc_m, g_m, sh_f, sc_f, g_f = jnp.split(mod[:, None, :], 6, axis=-1)
        modc = jax.nn.silu(c_ctx) @ ada_w[i] + ada_b[i]
        shc_m, scc_m, gc_m, shc_f, scc_f, gc_f = jnp.split(modc, 6, axis=-1)

        h = rms_norm(x, norm_mix[i]) * (1.0 + sc_m) + sh_m
        hc = rms_norm(xc, norm_mix[i]) * (1.0 + scc_m) + shc_m
        j = i // N_MIXERS
        if i % N_MIXERS == 0:
            out, out_c = diff_attention_mixer(h, hc, attn_w_qkv[j], attn_w_o[j], attn_q_norm[j],
                                              attn_k_norm[j], attn_sub_norm[j], attn_lambda[j],
                                              i, need_ctx)
        else:
            out, out_c = hgrn2_mixer(h, hc, hgrn_w_in[j], hgrn_w_o[j], hgrn_out_norm[j],
                                     lb_all[:, i], need_ctx)
        x = x + g_m * out

        f = rms_norm(x, norm_ffn[i]) * (1.0 + sc_f) + sh_f
        if need_ctx:
            xc = xc + gc_m * out_c
            fc = rms_norm(xc, norm_ffn[i]) * (1.0 + scc_f) + shc_f
            tokens = jnp.concatenate([f.reshape(-1, D), fc.reshape(-1, D)], axis=0)
            y = moe_ffn(tokens, router_w, router_bias, moe_w_gate[i], moe_w_up[i], moe_w_down[i])
            x = x + g_f * y[:B * T].reshape(B, T, D)
            xc = xc + gc_f * y[B * T:].reshape(xc.shape)
        else:
            y = moe_ffn(f.reshape(-1, D), router_w, router_bias, moe_w_gate[i], moe_w_up[i],
                        moe_w_down[i])
            x = x + g_f * y.reshape(B, T, D)
    return x
```

```python
import contextlib
import math

import numpy as np
import concourse.bass as bass
import concourse.mybir as mybir
from concourse.bass_utils import run_bass_kernel_spmd

F32 = mybir.dt.float32
BF16 = mybir.dt.bfloat16
AF = mybir.ActivationFunctionType
ALU = mybir.AluOpType
AX = mybir.AxisListType

D = 1024
NLAT = 2048
NCTX = 256
T = NLAT + NCTX
NT = T // 128
NE = 16
DFF = 512
EPS = 1e-6
BIG = 1.0e4


class _Op:
    __slots__ = ("eng", "fn", "deps", "is_dma", "slot", "ndma", "cum", "sig", "signals", "barriered")


class Prog:
    ENG = ("pe", "act", "dve", "pool", "sp")

    def __init__(self):
        self.nc = bass.Bass("TRN2", target_bir_lowering=False)
        self.ops = []
        self.last_w = {}
        self.readers = {}
        self.slot_cnt = {}
        self.last_on = {e: None for e in self.ENG}

    def _eng(self, e):
        nc = self.nc
        return {"pe": nc.tensor, "act": nc.scalar, "dve": nc.vector, "pool": nc.gpsimd, "sp": nc.sync}[e]

    def _deps(self, reads, writes):
        deps = set()
        for k in reads:
            w = self.last_w.get(k)
            if w is not None:
                deps.add(w)
        for k in writes:
            w = self.last_w.get(k)
            if w is not None:
                deps.add(w)
            for r in self.readers.get(k, ()):
                deps.add(r)
        return deps

    def _commit(self, oid, reads, writes):
        for k in reads:
            self.readers.setdefault(k, []).append(oid)
        for k in writes:
            self.last_w[k] = oid
            self.readers[k] = []

    def op(self, eng, fn, reads=(), writes=()):
        reads = list(reads)
        writes = list(writes)
        o = _Op()
        o.eng = eng
        o.fn = fn
        o.is_dma = False
        o.deps = self._deps(reads, writes)
        if eng == "pe":
            o.deps = {d for d in o.deps if self.ops[d].is_dma or self.ops[d].eng != "pe"}
        o.signals = False
        oid = len(self.ops)
        self.ops.append(o)
        self._commit(oid, reads, writes)
        self.last_on[eng] = oid
        return oid

    def dma(self, queue, slot, fn, reads=(), writes=(), ndma=1):
        reads = list(reads)
        writes = list(writes)
        o = _Op()
        o.eng = queue
        o.fn = fn
        o.is_dma = True
        o.slot = slot
        o.ndma = ndma
        o.barriered = False
        self.slot_cnt[slot] = self.slot_cnt.get(slot, 0) + ndma
        o.cum = self.slot_cnt[slot]
        o.deps = self._deps(reads, writes)
        o.signals = False
        oid = len(self.ops)
        self.ops.append(o)
        self._commit(oid, reads, writes)
        return oid

    def barrier(self):
        pend = set()
        for e in self.ENG:
            if self.last_on[e] is not None:
                pend.add(self.last_on[e])
        for i, o in enumerate(self.ops):
            if o.is_dma and not o.barriered:
                pend.add(i)
                o.barriered = True
        for e in self.ENG:
            o = _Op()
            o.eng = e
            o.fn = None
            o.is_dma = False
            o.deps = set(pend)
            o.signals = False
            self.ops.append(o)
        self.last_w = {}
        self.readers = {}

    def emit(self, final_wait_slots=()):
        nc = self.nc
        ops = self.ops
        for o in ops:
            for d in o.deps:
                if not ops[d].is_dma:
                    ops[d].signals = True
        self._stack = contextlib.ExitStack()
        esem = {e: self._stack.enter_context(nc.semaphore("s_" + e)) for e in self.ENG}
        ssem = {s: self._stack.enter_context(nc.semaphore("d_" + str(s))) for s in self.slot_cnt}
        cnt = {e: 0 for e in self.ENG}
        for o in ops:
            if not o.is_dma and o.signals:
                cnt[o.eng] += 1
                o.sig = cnt[o.eng]
        waited = {e: {} for e in self.ENG}
        for o in ops:
            eng = self._eng(o.eng)
            w = waited[o.eng]
            need = {}
            for d in o.deps:
                od = ops[d]
                if od.is_dma:
                    key = ("d", od.slot)
                    val = 16 * od.cum
                else:
                    key = ("e", od.eng)
                    val = od.sig
                if need.get(key, 0) < val:
                    need[key] = val
            for key, val in need.items():
                if w.get(key, 0) >= val:
                    continue
                sem = ssem[key[1]] if key[0] == "d" else esem[key[1]]
                eng.wait_ge(sem, val)
                w[key] = val
            if o.fn is None:
                continue
            if o.is_dma:
                insts = o.fn(eng)
                assert len(insts) == o.ndma, (len(insts), o.ndma)
                for ins in insts:
                    ins.then_inc(ssem[o.slot], 16)
            else:
                ins = o.fn(eng)
                if o.signals:
                    ins.then_inc(esem[o.eng], 1)
        for s in final_wait_slots:
            nc.sync.wait_ge(ssem[s], 16 * self.slot_cnt[s])
        return nc


class Builder:
    def __init__(self, stop=None, taps=()):
        self.p = Prog()
        self.nc = self.p.nc
        self.stop = stop
        self.taps = set(taps)
        self.tap_outs = []
        self.uid = 0

    def din(self, name, shape, dt=F32):
        return self.nc.dram_tensor(name, list(shape), dt, kind="ExternalInput").ap()

    def dout(self, name, shape, dt=F32):
        return self.nc.dram_tensor(name, list(shape), dt, kind="ExternalOutput").ap()

    def sb(self, st, name, shape, dt=F32):
        self.uid += 1
        return st.enter_context(self.nc.sbuf_tensor(f"{name}_{self.uid}", list(shape), dt))

    def tap(self, name, src_ap, shape, keys, dt=F32):
        if name not in self.taps:
            return
        o = self.dout("tap_" + name, shape, dt)
        self.p.dma("sp", "tap", lambda e: [e.dma_start(out=o, in_=src_ap)], reads=keys)
        self.tap_outs.append("tap_" + name)

    def A(self, out, in_, func, R, W, scale=None, bias=None, accum=None):
        kw = {}
        if scale is not None:
            kw["scale"] = scale
        if bias is not None:
            kw["bias"] = bias
        if accum is not None:
            kw["accum_out"] = accum
        self.p.op("act", lambda e: e.activation(out=out, in_=in_, func=func, **kw), R, W)

    def CP(self, eng, out, in_, R, W):
        if eng == "act":
            self.p.op("act", lambda e: e.copy(out=out, in_=in_), R, W)
        else:
            self.p.op(eng, lambda e: e.tensor_copy(out=out, in_=in_), R, W)

    def TT(self, eng, out, in0, in1, op, R, W):
        self.p.op(eng, lambda e: e.tensor_tensor(out=out, in0=in0, in1=in1, op=op), R, W)

    def TS(self, eng, out, in0, s1, s2, op0, op1, R, W):
        if s2 is None:
            self.p.op(eng, lambda e: e.tensor_scalar(out=out, in0=in0, scalar1=s1, scalar2=None, op0=op0), R, W)
        else:
            self.p.op(eng, lambda e: e.tensor_scalar(out=out, in0=in0, scalar1=s1, scalar2=s2, op0=op0, op1=op1), R, W)

    def STT(self, eng, out, in0, scalar, in1, op0, op1, R, W):
        self.p.op(eng, lambda e: e.scalar_tensor_tensor(out=out, in0=in0, scalar=scalar, in1=in1, op0=op0, op1=op1), R, W)

    def RED(self, out, in_, op, R, W):
        self.p.op("dve", lambda e: e.tensor_reduce(out=out, in_=in_, axis=AX.X, op=op), R, W)

    def RECIP(self, out, in_, R, W):
        self.p.op("dve", lambda e: e.reciprocal(out=out, in_=in_), R, W)

    def MEMSET(self, eng, ap, val, W):
        self.p.op(eng, lambda e: e.memset(ap, val), (), W)

    def MM(self, mms, R, W):
        mms = list(mms)

        def fn(e):
            ins = None
            for (o, l, r, s0, s1) in mms:
                ins = e.matmul(o, lhsT=l, rhs=r, start=s0, stop=s1)
            return ins
        self.p.op("pe", fn, R, W)

    def TR(self, trs, ident, R, W):
        trs = list(trs)

        def fn(e):
            ins = None
            for (o, i) in trs:
                ins = e.transpose(out=o, in_=i, identity=ident)
            return ins
        self.p.op("pe", fn, R, W)

    def DMA(self, queue, slot, pairs, R, W, slow=False):
        pairs = list(pairs)

        def fn(e):
            if slow:
                return [e.dma_start(out=o, in_=i, allow_slow_non_contiguous=True) for (o, i) in pairs]
            return [e.dma_start(out=o, in_=i) for (o, i) in pairs]
        self.p.dma(queue, slot, fn, R, W, ndma=len(pairs))


def tkeys(name, t0, t1):
    return [(name, t) for t in range(t0 // 128, (t1 + 127) // 128)]


def build(stop=None, taps=()):
    B = Builder(stop, taps)
    p, nc = B.p, B.nc
    A, CP, TT, TS, STT, RED, RECIP, MEMSET, MM, TR, DMA = B.A, B.CP, B.TT, B.TS, B.STT, B.RED, B.RECIP, B.MEMSET, B.MM, B.TR, B.DMA
    MUL, ADD, SUB, MAX, ISEQ = ALU.mult, ALU.add, ALU.subtract, ALU.max, ALU.is_equal

    xin = B.din("xin", [T, D])
    cc = B.din("cc", [2, D])
    ada_w = B.din("ada_w", [2, D, 6 * D])
    ada_b = B.din("ada_b", [2, 6 * D])
    norm_mix = B.din("norm_mix", [2, D])
    norm_ffn = B.din("norm_ffn", [2, D])
    w_qkv = B.din("attn_w_qkv", [D, 3 * D])
    w_ao = B.din("attn_w_o", [D, D])
    q_norm = B.din("attn_q_norm", [64])
    k_norm = B.din("attn_k_norm", [64])
    sub_norm = B.din("attn_sub_norm", [128])
    a_lambda = B.din("attn_lambda", [256])
    w_in = B.din("hgrn_w_in", [D, 5 * D])
    w_ho = B.din("hgrn_w_o", [D, D])
    out_norm = B.din("hgrn_out_norm", [128])
    lb_gamma = B.din("hgrn_lb_gamma", [2, 2, D])
    router_w = B.din("router_w", [D, NE])
    router_b = B.din("router_bias", [NE])
    w_gate = B.din("moe_w_gate", [2, NE, D, DFF])
    w_up = B.din("moe_w_up", [2, NE, D, DFF])
    w_down = B.din("moe_w_down", [2, NE, DFF, D])
    c_ident = B.din("cst_ident", [128, 128])
    c_blk = B.din("cst_blk", [128, 128])
    c_rm = B.din("cst_rm", [128, 128])
    c_cos = B.din("cst_cos", [128, NLAT])
    c_sin = B.din("cst_sin", [128, NLAT])
    c_maskf = B.din("cst_maskf", [64, 64])
    c_maskb = B.din("cst_maskb", [64, 64])
    yout = B.dout("y", [NLAT, D])

    top = contextlib.ExitStack()
    sb = B.sb
    X = sb(top, "X", [128, NT, D])
    HT = sb(top, "HT", [128, 8, T], BF16)
    G_l = sb(top, "G_l", [128, D])
    G_c = sb(top, "G_c", [128, D])
    IDENT = sb(top, "IDENT", [128, 128], BF16)
    ONESB = sb(top, "ONESB", [128, 128], BF16)
    BLK = sb(top, "BLK", [128, 128], BF16)
    RM = sb(top, "RM", [128, 128], BF16)
    EPSC = sb(top, "EPSC", [128, 1])
    CRL = sb(top, "CRL", [128, 8, 128], BF16)
    CRC = sb(top, "CRC", [128, 8, 128], BF16)
    GATES = sb(top, "GATES", [128, NT, NE])
    RB = sb(top, "RB", [128, NE])
    RWH = sb(top, "RWH", [128, 8, NE], BF16)
    RWL = sb(top, "RWL", [128, 8, NE], BF16)
    PB = [top.enter_context(nc.psum_tensor(f"PB{i}", [128, 512], F32)) for i in range(6)] + [None, None]
    PBHL = [None]
    PBk = [("PB", i) for i in range(8)]
    PBHk = ("PBH",)
    flex_ctr = [0]

    def flex_psum(st, all_f32):
        flex_ctr[0] += 1
        PB[6] = st.enter_context(nc.psum_tensor(f"PB6_{flex_ctr[0]}", [128, 512], F32))
        if all_f32:
            PB[7] = st.enter_context(nc.psum_tensor(f"PB7_{flex_ctr[0]}", [128, 512], F32))
            PBHL[0] = None
        else:
            PB[7] = None
            PBHL[0] = st.enter_context(nc.psum_tensor(f"PBH_{flex_ctr[0]}", [128, 1024], BF16))

    with contextlib.ExitStack() as st:
        C32 = sb(st, "C32", [128, 3, 128])
        CCT = sb(st, "CCT", [128, 8, 2])
        SCT = sb(st, "SCT", [128, 8, 2])
        RW32 = sb(st, "RW32", [128, 8, NE])
        RWT = sb(st, "RWT", [128, 8, NE])
        DMA("sp", "cst0", [(C32[:, 0, :], c_ident), (C32[:, 1, :], c_blk), (C32[:, 2, :], c_rm)], [], ["C32"])
        DMA("sp", "cst1", [(CCT[:, :, 0], cc[0].rearrange("(c p) -> p c", p=128)),
                          (CCT[:, :, 1], cc[1].rearrange("(c p) -> p c", p=128))], [], ["CCT"], slow=True)
        DMA("sp", "cst2", [(RW32[:], router_w.rearrange("(c p) e -> p c e", p=128)),
                          (RB[:], router_b.partition_broadcast(128))], [], ["RW32", "RB"])
        CP("dve", IDENT[:], C32[:, 0, :], ["C32"], ["IDENT"])
        CP("dve", BLK[:], C32[:, 1, :], ["C32"], ["BLK"])
        CP("dve", RM[:], C32[:, 2, :], ["C32"], ["RM"])
        MEMSET("dve", ONESB[:], 1.0, ["ONESB"])
        MEMSET("dve", EPSC[:], EPS, ["EPSC"])
        A(SCT[:], CCT[:], AF.Silu, ["CCT"], ["SCT"])
        CP("dve", CRL[:], SCT[:, :, 0:1].to_broadcast([128, 8, 128]), ["SCT"], ["CRL"])
        CP("dve", CRC[:], SCT[:, :, 1:2].to_broadcast([128, 8, 128]), ["SCT"], ["CRC"])
        CP("dve", RWH[:], RW32[:], ["RW32"], ["RWH"])
        TT("dve", RWT[:], RW32[:], RWH[:], SUB, ["RW32", "RWH"], ["RWT"])
        CP("dve", RWL[:], RWT[:], ["RWT"], ["RWL"])
        xv = xin.rearrange("(n p) d -> p n d", p=128)
        for g in range(0, NT, 3):
            DMA("sp", f"xld{g}", [(X[:, g:g + 3, :], xv[:, g:g + 3, :])], [], [("X", t) for t in range(g, g + 3)])
        p.barrier()

    def modslice(ADAW, BIAS, layer, j, out_l, out_c, defer=False):
        DMA("sp", "adab", [(BIAS[:], ada_b[layer, j * D:(j + 1) * D].partition_broadcast(128))], [], ["BIAS"])
        for half in range(2):
            cs_ = slice(j * D + half * 512, j * D + (half + 1) * 512)
            DMA("pool", f"adaw{half}", [(ADAW[half][:], ada_w[layer, :, cs_].rearrange("(c p) n -> p c n", p=128))], [], [("ADAW", half)])
        def compute():
            k = 0
            for half in range(2):
                for CR, crk, out in ((CRL, "CRL", out_l), (CRC, "CRC", out_c)):
                    if out is None:
                        continue
                    bank = k % 2
                    k += 1
                    MM([(PB[bank][:], CR[:, c, :], ADAW[half][:, c, :], c == 0, c == 7) for c in range(8)],
                       [crk, ("ADAW", half)], [PBk[bank]])
                    TT("dve", out[0][:, half * 512:(half + 1) * 512], PB[bank][:], BIAS[:, half * 512:(half + 1) * 512], ADD,
                       [PBk[bank], "BIAS"], [(out[1], half)])
        if defer:
            return compute
        compute()
        return None

    def norm_phase(layer, which, ntiles):
        j0 = 0 if which == "mix" else 3
        nw = norm_mix if which == "mix" else norm_ffn
        with_ctx = ntiles > 16
        with contextlib.ExitStack() as st:
            flex_psum(st, False)
            PBH = PBHL[0]
            ADAW = [sb(st, f"ADAW{i}", [128, 8, 512], BF16) for i in range(2)]
            BIAS = sb(st, "BIAS", [128, D])
            GN = sb(st, "GN", [128, D])
            SC_l = sb(st, "SC_l", [128, D])
            SC_c = sb(st, "SC_c", [128, D])
            A_l = sb(st, "A_l", [128, D])
            A_c = sb(st, "A_c", [128, D])
            SH_l = sb(st, "SH_l", [128, D])
            SH_c = sb(st, "SH_c", [128, D])
            TMP = [sb(st, f"TMP{i}", [128, D]) for i in range(2)]
            HB = [sb(st, f"HB{i}", [128, D], BF16) for i in range(2)]
            JUNK = HB[1]
            SS = sb(st, "SS", [128, NT])
            RSTD = sb(st, "RSTD", [128, NT])
            if which == "ffn":
                H32 = [sb(st, f"H32{i}", [128, D]) for i in range(2)]
                HLO = [sb(st, f"HLO{i}", [128, D], BF16) for i in range(2)]
                HTLO = [sb(st, f"HTLO{i}", [128, 8, 128], BF16) for i in range(2)]
                LG = sb(st, "LG", [128, NT, NE])
                RT = {n: sb(st, f"RT_{n}", [128, NT, NE]) for n in ("AFF", "BI", "EQ", "MB", "E1")}
                RS = {n: sb(st, f"RS_{n}", [128, NT, 4]) for n in ("M1", "M2", "GS", "PEN")}
                RV = {n: sb(st, f"RV_{n}", [128, NT]) for n in ("GM", "V1", "V2", "SUM")}
            DMA("sp", "gn", [(GN[:], nw[layer].partition_broadcast(128))], [], ["GN"])
            modslice(ADAW, BIAS, layer, j0 + 0, (SH_l, "SH_l"), (SH_c, "SH_c") if with_ctx else None)
            modslice(ADAW, BIAS, layer, j0 + 1, (SC_l, "SC_l"), (SC_c, "SC_c") if with_ctx else None)
            STT("dve", A_l[:], SC_l[:], 1.0, GN[:], ADD, MUL, [("SC_l", 0), ("SC_l", 1), "GN"], ["A_l"])
            if with_ctx:
                STT("dve", A_c[:], SC_c[:], 1.0, GN[:], ADD, MUL, [("SC_c", 0), ("SC_c", 1), "GN"], ["A_c"])
            gate_compute = modslice(ADAW, BIAS, layer, j0 + 2, (G_l, "G_l"), (G_c, "G_c") if with_ctx else None, defer=True)
            MEMSET("dve", SS[:], 0.0, ["SS"])
            for t in range(ntiles):
                A(JUNK[:], X[:, t, :], AF.Square, [("X", t), "SS"], [("HB", 1), "SS"], accum=SS[:, t:t + 1])
            A(RSTD[:, 0:ntiles], SS[:, 0:ntiles], AF.Sqrt, ["SS", "EPSC"], ["RSTD"], scale=1.0 / D, bias=EPSC[:, 0:1])
            RECIP(RSTD[:, 0:ntiles], RSTD[:, 0:ntiles], ["RSTD"], ["RSTD"])
            def stage_a(t):
                i = t % 2
                lat = t < 16
                Aw, Ak = (A_l, "A_l") if lat else (A_c, "A_c")
                Sw, Sk = (SH_l, "SH_l") if lat else (SH_c, "SH_c")
                Sks = [(Sk, 0), (Sk, 1)]
                STT("dve", TMP[i][:], X[:, t, :], RSTD[:, t:t + 1], Aw[:], MUL, MUL, [("X", t), "RSTD", Ak], [("TMP", i)])
                if which == "ffn" and t >= 1:
                    stage_b(t - 1)
                if which == "mix":
                    TT("pool" if t % 2 == 0 else "dve", HB[i][:], TMP[i][:], Sw[:], ADD, [("TMP", i)] + Sks, [("HB", i)])
                else:
                    TT("pool", H32[i][:], TMP[i][:], Sw[:], ADD, [("TMP", i)] + Sks, [("H32", i)])
                    CP("act", HB[i][:], H32[i][:], [("H32", i)], [("HB", i)])
                    TT("dve", HLO[i][:], H32[i][:], HB[i][:], SUB, [("H32", i), ("HB", i)], [("HLO", i)])
                TR([(PBH[:, c * 128:(c + 1) * 128], HB[i][:, c * 128:(c + 1) * 128]) for c in range(8)], IDENT[:],
                   [("HB", i), "IDENT"], [PBHk])
                CP("act", HT[:, :, t * 128:(t + 1) * 128], PBH[:].rearrange("p (c j) -> p c j", c=8), [PBHk], [("HT", t)])
                if which == "ffn":
                    TR([(PBH[:, c * 128:(c + 1) * 128], HLO[i][:, c * 128:(c + 1) * 128]) for c in range(8)], IDENT[:],
                       [("HLO", i), "IDENT"], [PBHk])
                    CP("act", HTLO[i][:], PBH[:].rearrange("p (c j) -> p c j", c=8), [PBHk], [("HTLO", i)])

            def stage_b(t):
                i = t % 2
                lg = PB[2 + i][:, 0:NE]
                mms = []
                for c in range(8):
                    mms.append((lg, HT[:, c, t * 128:(t + 1) * 128], RWH[:, c, :], c == 0, False))
                    mms.append((lg, HTLO[i][:, c, :], RWH[:, c, :], False, False))
                    mms.append((lg, HT[:, c, t * 128:(t + 1) * 128], RWL[:, c, :], False, c == 7))
                MM(mms, [("HT", t), ("HTLO", i), "RWH", "RWL"], [PBk[2 + i]])
                CP("act", LG[:, t, :], lg, [PBk[2 + i]], [("LG", t)])

            def router_all(nt):
                f3 = lambda ap: ap[:, 0:nt, :]
                v4 = lambda ap: ap[:, 0:nt, :].rearrange("p t (g j) -> p t g j", g=4)
                g3 = lambda ap: ap[:, 0:nt, :]
                v2 = lambda ap: ap[:, 0:nt]
                bj = lambda ap: ap[:, 0:nt, :].unsqueeze(3).to_broadcast([128, nt, 4, 4])
                be = lambda ap: ap[:, 0:nt].unsqueeze(2).to_broadcast([128, nt, NE])
                bg4 = lambda ap: ap[:, 0:nt].unsqueeze(2).to_broadcast([128, nt, 4])
                AFF, BI, EQ, MB, E1 = (RT[n] for n in ("AFF", "BI", "EQ", "MB", "E1"))
                E2 = EQ
                M1, M2, GS, PEN = (RS[n] for n in ("M1", "M2", "GS", "PEN"))
                GM, V1, V2, SUM = (RV[n] for n in ("GM", "V1", "V2", "SUM"))
                A(f3(AFF), f3(LG), AF.Sigmoid, [("LG", t) for t in range(nt)], ["AFF"])
                TT("dve", f3(BI), f3(AFF), RB[:].unsqueeze(1).to_broadcast([128, nt, NE]), ADD, ["AFF", "RB"], ["BI"])
                RED(g3(M1), v4(BI), MAX, ["BI"], ["M1"])
                TT("dve", v4(EQ), v4(BI), bj(M1), ISEQ, ["BI", "M1"], ["EQ"])
                STT("dve", f3(EQ), f3(EQ), -BIG, f3(BI), MUL, ADD, ["EQ", "BI"], ["EQ"])
                RED(g3(M2), v4(EQ), MAX, ["EQ"], ["M2"])
                TT("dve", g3(GS), g3(M1), g3(M2), ADD, ["M1", "M2"], ["GS"])
                RED(v2(GM), g3(GS), MAX, ["GS"], ["GM"])
                TT("dve", g3(M1), g3(GS), bg4(GM), ISEQ, ["GS", "GM"], ["M1"])
                TS("dve", g3(PEN), g3(M1), -1.0, BIG, ADD, MUL, ["M1"], ["PEN"])
                TT("dve", v4(MB), v4(BI), bj(PEN), ADD, ["BI", "PEN"], ["MB"])
                RED(v2(V1), f3(MB), MAX, ["MB"], ["V1"])
                TT("dve", f3(E1), f3(MB), be(V1), ISEQ, ["MB", "V1"], ["E1"])
                STT("dve", f3(MB), f3(E1), -BIG, f3(MB), MUL, ADD, ["E1", "MB"], ["MB"])
                RED(v2(V2), f3(MB), MAX, ["MB"], ["V2"])
                TT("dve", f3(E2), f3(MB), be(V2), ISEQ, ["MB", "V2"], ["EQ"])
                TT("dve", f3(E1), f3(E1), f3(E2), ADD, ["E1", "EQ"], ["E1"])
                TT("dve", f3(E1), f3(AFF), f3(E1), MUL, ["AFF", "E1"], ["E1"])
                RED(v2(SUM), f3(E1), ADD, ["E1"], ["SUM"])
                RECIP(v2(SUM), v2(SUM), ["SUM"], ["SUM"])
                TT("dve", GATES[:, 0:nt, :], f3(E1), be(SUM), MUL, ["E1", "SUM"], [("GATES", t) for t in range(nt)])

            for t in range(ntiles):
                stage_a(t)
            gate_compute()
            if which == "ffn":
                stage_b(ntiles - 1)
                router_all(ntiles)
            p.barrier()


    def attention_phase():
        blocks = [(0, 512), (512, 1024), (1024, 1536), (1536, 2048), (2048, 2304)]
        with contextlib.ExitStack() as st:
            flex_psum(st, True)
            CS = [sb(st, f"CS{i}", [128, 2, 512]) for i in range(1)]
            WQKV = sb(st, "WQKV", [128, 8, 3, 128], BF16)
            WO = [sb(st, f"WO{i}", [128, D], BF16) for i in range(2)]
            WOG = [[sb(st, f"WOG{i}{g}", [128, D], BF16) for g in range(2)] for i in range(2)]
            QT = [sb(st, f"QT{i}", [128, T], BF16) for i in range(2)]
            KT = [sb(st, f"KT{i}", [128, T], BF16) for i in range(2)]
            VT = [sb(st, f"VT{i}", [128, NT, 128], BF16) for i in range(2)]
            ONT = sb(st, "ONT", [128, T], BF16)
            GQ = sb(st, "GQ", [128, 1])
            GK = sb(st, "GK", [128, 1])
            GSUB = sb(st, "GSUB", [128, 1])
            LAMB = sb(st, "LAMB", [128, 256])
            LTMP = sb(st, "LTMP", [128, 128])
            LS = sb(st, "LS", [128, 2])
            NLAM = sb(st, "NLAM", [128, 1])
            SQ = sb(st, "SQ", [128, 512], BF16)
            RST = sb(st, "RST", [128, 512])
            QN = sb(st, "QN", [128, 512], BF16)
            T1 = sb(st, "T1", [128, 512])
            T2 = sb(st, "T2", [128, 512])
            E = [sb(st, f"E{i}", [128, 512], BF16) for i in range(4)]
            ZS = [[sb(st, f"ZS{b}{i}", [128, 512]) for i in range(2)] for b in range(2)]
            OS = [[sb(st, f"OS{b}{i}", [128, 512]) for i in range(2)] for b in range(2)]

            col = lambda v: v.rearrange("(p o) -> p o", o=1)
            DMA("sp", "acst0", [(GQ[0:64, :], col(q_norm)), (GQ[64:128, :], col(q_norm)), (GK[0:64, :], col(k_norm)),
                                (GK[64:128, :], col(k_norm)), (GSUB[:], col(sub_norm)),
                                (LAMB[:], a_lambda.partition_broadcast(128))], [], ["GQ", "GK", "GSUB", "LAMB"])
            lam_init0 = 0.8 - 0.6 * math.exp(0.0)
            TS("dve", GSUB[:], GSUB[:], 1.0 - lam_init0, None, MUL, None, ["GSUB"], ["GSUB"])
            lv = LAMB[:].rearrange("p (a b d) -> p a b d", a=2, b=2)
            TT("dve", LTMP[:].rearrange("p (a d) -> p a d", a=2), lv[:, :, 0, :], lv[:, :, 1, :], MUL, ["LAMB"], ["LTMP"])
            RED(LS[:], LTMP[:].rearrange("p (a d) -> p a d", a=2), ADD, ["LTMP"], ["LS"])
            A(LS[:], LS[:], AF.Exp, ["LS"], ["LS"])
            TT("dve", NLAM[:], LS[:, 1:2], LS[:, 0:1], SUB, ["LS"], ["NLAM"])
            TS("dve", NLAM[:], NLAM[:], -lam_init0, None, ADD, None, ["NLAM"], ["NLAM"])

            cnt = {"e": 0, "cs": 0}
            qdone = {}

            def gen_proj(h):
                par = h % 2
                DMA("pool", "wqkv", [(WQKV[:, :, s_, :], w_qkv[:, s_ * D + h * 128:s_ * D + (h + 1) * 128]
                                      .rearrange("(c p) f -> p c f", p=128)) for s_ in range(3)], [], ["WQKV"])
                DMA("pool", f"wo{par}", [(WO[par][:], w_ao[h * 128:(h + 1) * 128, :])], [], [("WO", par)])
                yield

                def qk(s_, dst, gcol, gk, dkey, t0, t1):
                    n = t1 - t0
                    pq = PB[2][:, 0:n]
                    MM([(pq, WQKV[:, c, s_, :], HT[:, c, t0:t1], c == 0, c == 7) for c in range(8)],
                       ["WQKV"] + tkeys("HT", t0, t1), [PBk[2]])
                    yield
                    A(SQ[:, 0:n], pq, AF.Square, [PBk[2]], ["SQ"])
                    yield
                    yield
                    MM([(PB[3][:, 0:n], BLK[:], SQ[:, 0:n], True, True)], ["BLK", "SQ"], [PBk[3]])
                    A(RST[:, 0:n], PB[3][:, 0:n], AF.Ln, [PBk[3], "EPSC"], ["RST"], scale=1.0 / 64, bias=EPSC[:, 0:1])
                    A(RST[:, 0:n], RST[:, 0:n], AF.Exp, ["RST"], ["RST"], scale=-0.5)
                    yield
                    if t0 < NLAT:
                        cs = 0
                        DMA("sp", f"cs{cs}", [(CS[cs][:, 0, 0:n], c_cos[:, t0:t1]), (CS[cs][:, 1, 0:n], c_sin[:, t0:t1])], [], [("CS", cs)])
                        STT("dve", QN[:, 0:n], pq, gcol[:, 0:1], RST[:, 0:n], MUL, MUL, [PBk[2], gk, "RST"], ["QN"])
                        yield
                        yield
                        MM([(PB[3][:, 0:n], RM[:], QN[:, 0:n], True, True)], ["RM", "QN"], [PBk[3]])
                        TT("pool", T1[:, 0:n], QN[:, 0:n], CS[cs][:, 0, 0:n], MUL, ["QN", ("CS", cs)], ["T1"])
                        yield
                        TT("dve", T2[:, 0:n], PB[3][:, 0:n], CS[cs][:, 1, 0:n], MUL, [PBk[3], ("CS", cs)], ["T2"])
                        TT("pool", dst[:, t0:t1], T1[:, 0:n], T2[:, 0:n], ADD, ["T1", "T2"], tkeys(dkey, t0, t1))
                        yield True
                    else:
                        STT("dve", dst[:, t0:t1], pq, gcol[:, 0:1], RST[:, 0:n], MUL, MUL, [PBk[2], gk, "RST"], tkeys(dkey, t0, t1))
                        yield True
                for (t0, t1) in blocks:
                    yield from qk(1, KT[par], GK, "GK", ("KT", par), t0, t1)
                for g in range(0, NT, 4):
                    ng = min(4, NT - g)
                    mms = []
                    for k_ in range(ng):
                        tt = g + k_
                        mms += [(PB[2][:, k_ * 128:(k_ + 1) * 128], HT[:, c, tt * 128:(tt + 1) * 128], WQKV[:, c, 2, :], c == 0, c == 7)
                                for c in range(8)]
                    MM(mms, ["WQKV"] + [("HT", g + k_) for k_ in range(ng)], [PBk[2]])
                    yield
                    CP("dve", VT[par][:, g:g + ng, :], PB[2][:, 0:ng * 128].rearrange("p (a e) -> p a e", a=ng), [PBk[2]],
                       [(("VT", par), g + k_) for k_ in range(ng)])
                    yield True
                TT("pool", WOG[par][0][:], WO[par][:], G_l[:], MUL, [("WO", par), "G_l"], [("WOG", par, 0)])
                yield True
                TT("pool", WOG[par][1][:], WO[par][:], G_c[:], MUL, [("WO", par), "G_c"], [("WOG", par, 1)])
                yield True
                for bi, (t0, t1) in enumerate(blocks):
                    yield from qk(0, QT[par], GQ, "GQ", ("QT", par), t0, t1)

            def gen_wo(h):
                par = h % 2
                for t in range(NT):
                    gi = 0 if t < 16 else 1
                    for dbl in range(2):
                        ds = slice(dbl * 512, (dbl + 1) * 512)
                        b_ = 2 + dbl
                        MM([(PB[b_][:], ONT[:, t * 128:(t + 1) * 128], WOG[par][gi][:, ds], True, True)], [("ONT", t), ("WOG", par, gi)], [PBk[b_]])
                        TT("dve", X[:, t, ds], X[:, t, ds], PB[b_][:], ADD, [("X", t), PBk[b_]], [("X", t)])
                        yield True

            def gen_attn(h):
                par = h % 2
                for bi, (q0, q1) in enumerate(blocks):
                    nq = q1 - q0
                    ktiles = list(range(NT)) if q0 < NLAT else [16, 17]
                    pend = []

                    def av(ki, kt, ebs):
                        first, last = ki == 0, ki == len(ktiles) - 1
                        MM([(PB[4][:, 0:nq], VT[par][:, kt, :], E[ebs[0]][:, 0:nq], first, last),
                            (PB[5][:, 0:nq], VT[par][:, kt, :], E[ebs[1]][:, 0:nq], first, last),
                            (PB[6][:, 0:nq], ONESB[:], E[ebs[0]][:, 0:nq], first, last),
                            (PB[7][:, 0:nq], ONESB[:], E[ebs[1]][:, 0:nq], first, last)],
                           [(("VT", par), kt), ("E", ebs[0]), ("E", ebs[1]), "ONESB"], [PBk[4], PBk[5], PBk[6], PBk[7]])
                    for ki, kt in enumerate(ktiles):
                        ebs = (cnt["e"] % 4, (cnt["e"] + 1) % 4)
                        cnt["e"] += 2
                        ks = slice(kt * 128, (kt + 1) * 128)
                        for m in range(2):
                            MM([(PB[m][:, 0:nq], KT[par][m * 64:(m + 1) * 64, ks], QT[par][m * 64:(m + 1) * 64, q0:q1], True, True)],
                               [(("KT", par), kt)] + tkeys(("QT", par), q0, q1), [PBk[m]])
                            A(E[ebs[m]][:, 0:nq], PB[m][:, 0:nq], AF.Exp, [PBk[m]], [("E", ebs[m])], scale=0.125)
                        if pend:
                            av(*pend.pop())
                        pend.append((ki, kt, ebs))
                        yield
                    qdone[h] = bi + 1
                    av(*pend.pop())
                    yield
                    zb = bi % 2
                    for m in range(2):
                        CP("dve", ZS[zb][m][:, 0:nq], PB[6 + m][:, 0:nq], [PBk[6 + m]], [("ZS", zb, m)])
                        CP("act", OS[zb][m][:, 0:nq], PB[4 + m][:, 0:nq], [PBk[4 + m]], [("OS", zb, m)])
                    prio.append(gen_epi(zb, q0, q1))
                    yield

            def gen_epi(zb, q0, q1):
                nq = q1 - q0
                Z, O = ZS[zb], OS[zb]
                for m in range(2):
                    A(Z[m][:, 0:nq], Z[m][:, 0:nq], AF.Ln, [("ZS", zb, m)], [("ZS", zb, m)])
                    A(Z[m][:, 0:nq], Z[m][:, 0:nq], AF.Exp, [("ZS", zb, m)], [("ZS", zb, m)], scale=-1.0)
                    yield
                    TT("pool", O[m][:, 0:nq], O[m][:, 0:nq], Z[m][:, 0:nq], MUL, [("OS", zb, m), ("ZS", zb, m)], [("OS", zb, m)])
                    yield
                STT("dve", O[0][:, 0:nq], O[1][:, 0:nq], NLAM[:, 0:1], O[0][:, 0:nq], MUL, ADD, [("OS", zb, 0), ("OS", zb, 1), "NLAM"], [("OS", zb, 0)])
                yield
                A(SQ[:, 0:nq], O[0][:, 0:nq], AF.Square, [("OS", zb, 0)], ["SQ"])
                yield
                yield
                MM([(PB[3][:, 0:nq], ONESB[:], SQ[:, 0:nq], True, True)], ["ONESB", "SQ"], [PBk[3]])
                A(RST[:, 0:nq], PB[3][:, 0:nq], AF.Ln, [PBk[3], "EPSC"], ["RST"], scale=1.0 / 128, bias=EPSC[:, 0:1])
                A(RST[:, 0:nq], RST[:, 0:nq], AF.Exp, ["RST"], ["RST"], scale=-0.5)
                yield
                STT("dve", ONT[:, q0:q1], O[0][:, 0:nq], GSUB[:, 0:1], RST[:, 0:nq], MUL, MUL, [("OS", zb, 0), "GSUB", "RST"], tkeys("ONT", q0, q1))
                yield

            bgq = []
            prio = []
            BG_PER_MAIN = 2

            rs_state = {"cur": None, "cur_prio": False}

            def run_stage(main, drain_all=False):
                main_alive = main is not None
                cur, cur_prio = rs_state["cur"], rs_state["cur_prio"]
                while main_alive or bgq or (cur is not None and not cur_prio) or (drain_all and (prio or cur is not None)):
                    if main_alive:
                        try:
                            next(main)
                        except StopIteration:
                            main_alive = False
                    for _rep in range(BG_PER_MAIN):
                        if cur is None:
                            if prio:
                                cur, cur_prio = prio.pop(0), True
                            elif bgq:
                                cur, cur_prio = bgq[0], False
                        if cur is not None:
                            try:
                                r = next(cur)
                            except StopIteration:
                                if not cur_prio:
                                    bgq.pop(0)
                                cur = None
                                continue
                            if r is True and not cur_prio and prio:
                                cur = None
                rs_state["cur"], rs_state["cur_prio"] = cur, cur_prio

            bgq.append(gen_proj(0))
            run_stage(None)
            for h in range(8):
                if h > 0:
                    bgq.append(gen_wo(h - 1))
                if h + 1 < 8:
                    bgq.append(gen_proj(h + 1))
                run_stage(gen_attn(h))
            bgq.append(gen_wo(7))
            run_stage(None, drain_all=True)
            p.barrier()

    def moe_phase(layer, ntiles):
        blocks = [(0, 512), (512, 1024), (1024, 1536), (1536, 2048)] + ([(2048, 2304)] if ntiles > 16 else [])
        with contextlib.ExitStack() as st:
            flex_psum(st, False)
            WG = [sb(st, f"WG{i}", [128, 8, DFF], BF16) for i in range(2)]
            WU = [sb(st, f"WU{i}", [128, 8, DFF], BF16) for i in range(2)]
            WD = [sb(st, f"WD{i}", [128, 4, D], BF16) for i in range(2)]
            SG = [sb(st, f"SG{i}", [128, 512]) for i in range(2)]
            HID = [sb(st, f"HID{i}", [128, 4, 512], BF16) for i in range(2)]
            WT = [sb(st, f"WT{i}", [128, 512]) for i in range(2)]

            def load(e):
                i = e % 2
                DMA("pool", f"wg{i}", [(WG[i][:], w_gate[layer, e].rearrange("(c p) f -> p c f", p=128))], [], [("WG", i)])
                DMA("pool", f"wu{i}", [(WU[i][:], w_up[layer, e].rearrange("(c p) f -> p c f", p=128))], [], [("WU", i)])
                DMA("pool", f"wd{i}", [(WD[i][:], w_down[layer, e].rearrange("(c p) n -> p c n", p=128))], [], [("WD", i)])
            load(0)
            cnt = {"c2": 0, "c3": 0}

            def gate_up(e, blk, hb):
                i = e % 2
                (t0, t1) = blk
                n = t1 - t0
                for ffc in range(4):
                    j = cnt["c2"] % 2
                    cnt["c2"] += 1
                    fs = slice(ffc * 128, (ffc + 1) * 128)
                    MM([(PB[j][:, 0:n], WG[i][:, c, fs], HT[:, c, t0:t1], c == 0, c == 7) for c in range(8)],
                       [("WG", i)] + tkeys("HT", t0, t1), [PBk[j]])
                    MM([(PB[2 + j][:, 0:n], WU[i][:, c, fs], HT[:, c, t0:t1], c == 0, c == 7) for c in range(8)],
                       [("WU", i)] + tkeys("HT", t0, t1), [PBk[2 + j]])
                    A(SG[j][:, 0:n], PB[j][:, 0:n], AF.Silu, [PBk[j]], [("SG", j)])
                    TT("dve", HID[hb][:, ffc, 0:n], SG[j][:, 0:n], PB[2 + j][:, 0:n], MUL, [("SG", j), PBk[2 + j]], [("HID", hb)])

            def down(e, blk, hb):
                i = e % 2
                (t0, t1) = blk
                for tt in range(t0 // 128, t1 // 128):
                    Gw, Gk_ = (G_l, "G_l") if tt < 16 else (G_c, "G_c")
                    o0 = tt * 128 - t0
                    for dbl in range(2):
                        b_ = 4 + (cnt["c3"] % 3)
                        w_ = cnt["c3"] % 2
                        cnt["c3"] += 1
                        ds = slice(dbl * 512, (dbl + 1) * 512)
                        MM([(PB[b_][:], HID[hb][:, ffc, o0:o0 + 128], WD[i][:, ffc, ds], ffc == 0, ffc == 3) for ffc in range(4)],
                           [("HID", hb), ("WD", i)], [PBk[b_]])
                        STT("dve", WT[w_][:], PB[b_][:], GATES[:, tt, e:e + 1], Gw[:, ds], MUL, MUL,
                            [PBk[b_], ("GATES", tt), Gk_], [("WT", w_)])
                        TT("pool", X[:, tt, ds], X[:, tt, ds], WT[w_][:], ADD, [("X", tt), ("WT", w_)], [("X", tt)])
                    if layer == 1 and e == NE - 1 and tt < 16:
                        DMA("sp", "yout", [(yout.rearrange("(n p) d -> p n d", p=128)[:, tt:tt + 1, :], X[:, tt:tt + 1, :])], [("X", tt)], [])
                        out_streamed[0] = True

            items = [(e, blk) for e in range(NE) for blk in blocks]
            for k_, (e, blk) in enumerate(items):
                gate_up(e, blk, k_ % 2)
                if k_ >= 1:
                    pe_, pblk = items[k_ - 1]
                    down(pe_, pblk, (k_ - 1) % 2)
                if blk == blocks[0] and e + 1 < NE:
                    load(e + 1)
            pe_, pblk = items[-1]
            down(pe_, pblk, (len(items) - 1) % 2)
            p.barrier()

    def hgrn_phase():
        BL = 256
        NCH = BL // 64
        lat_blocks = [(b * BL, (b + 1) * BL) for b in range(NLAT // BL)]
        ctx_block = (NLAT, T)
        NB = len(lat_blocks)
        with contextlib.ExitStack() as st:
            flex_psum(st, False)
            PBH = PBHL[0]
            WIN = [sb(st, f"WIN{i}", [128, 8, 5, 128], BF16) for i in range(2)]
            WO = [sb(st, f"HWO{i}", [128, D], BF16) for i in range(2)]
            WOG = [sb(st, f"HWOG{i}", [128, D], BF16) for i in range(2)]
            GAM = sb(st, "GAM", [128, 8, 2, 2])
            LBT = sb(st, "LBT", [128, 8, 2])
            OML = sb(st, "OML", [128, 8, 2])
            LBM1 = sb(st, "LBM1", [128, 8, 2])
            GOUT = sb(st, "GOUT", [128, 1])
            ONEC = sb(st, "ONEC", [128, 1])
            MASK = [sb(st, f"MASK{i}", [64, 64]) for i in range(2)]
            ONES32 = sb(st, "ONES32", [128, BL])
            QS = sb(st, "QS", [128, T])
            VCH = sb(st, "VCH", [64, T // 64, 128], BF16)
            OT = X[:, 16:18, :].rearrange("p a d -> p (a d)")
            ONT = [sb(st, f"HONT{i}", [128, NLAT], BF16) for i in range(2)]
            VTF = [sb(st, f"VTF{i}", [128, BL], BF16) for i in range(2)]
            S = [sb(st, f"S{i}", [128, 128]) for i in range(2)]
            tmp = lambda nm, shape, dt=F32: [sb(st, f"{nm}{i}", shape, dt) for i in range(2)]
            SBF = tmp("SBF", [128, NCH, 128], BF16)
            SG = tmp("HSG", [128, BL])
            KK = tmp("KK", [128, BL])
            CIN = tmp("CIN", [128, BL])
            D1 = tmp("D1", [128, BL])
            D2 = tmp("D2", [128, BL])
            QB = tmp("QB", [128, BL], BF16)
            QTL = tmp("QTL", [128, BL], BF16)
            KTL = tmp("KTL", [128, BL], BF16)
            KBT = tmp("KBT", [128, BL], BF16)
            KBCH = tmp("KBCH", [64, NCH, 128], BF16)
            AT = tmp("AT", [64, BL], BF16)
            PIN = tmp("PIN", [128, NCH])
            DCH = tmp("DCH", [128, NCH])
            TOT = tmp("TOT", [128, 1])
            SGG = tmp("SGG", [128, BL])
            SQ = tmp("HSQ", [128, BL], BF16)
            RS = tmp("HRS", [128, BL])

            for a_ in range(2):
                for l_ in range(2):
                    DMA("sp", f"hcst{a_}{l_}", [(GAM[:, :, a_, l_], lb_gamma[a_, l_].rearrange("(h p) -> p h", p=128))], [], ["GAM"], slow=True)
            DMA("sp", "hcstm", [(GOUT[:], out_norm.rearrange("(p o) -> p o", o=1)), (MASK[0][:], c_maskf), (MASK[1][:], c_maskb)],
                [], ["GOUT", "MASK"])
            TT("dve", LBT[:], GAM[:, :, :, 1], GAM[:, :, :, 0], SUB, ["GAM"], ["LBT"])
            MEMSET("dve", ONEC[:], 1.0, ["ONEC"])
            A(LBT[:], LBT[:], AF.Exp, ["LBT"], ["LBT"], scale=-1.0)
            A(LBT[:], LBT[:], AF.Ln, ["LBT", "ONEC"], ["LBT"], bias=ONEC[:, 0:1])
            A(LBT[:], LBT[:], AF.Exp, ["LBT"], ["LBT"], scale=-1.0)
            TS("dve", OML[:], LBT[:], -1.0, 1.0, MUL, ADD, ["LBT"], ["OML"])
            TS("dve", LBM1[:], LBT[:], -1.0, None, ADD, None, ["LBT"], ["LBM1"])
            MEMSET("dve", ONES32[:], 1.0, ["ONES32"])
            MEMSET("dve", PB[4][:], 0.0, [PBk[4]])
            MEMSET("dve", PB[5][:], 0.0, [PBk[5]])

            def load(h):
                i = h % 2
                DMA("pool", f"win{i}", [(WIN[i][:, :, s_, :], w_in[:, s_ * D + h * 128:s_ * D + (h + 1) * 128]
                                         .rearrange("(c p) f -> p c f", p=128)) for s_ in range(5)], [], [("WIN", i)])
                DMA("pool", f"hwo{i}", [(WO[i][:], w_ho[h * 128:(h + 1) * 128, :])], [], [("HWO", i)])
            load(0)
            cnt = {"pj": 0, "w": 0}

            PJB = [0, 1, 6]

            def proj(i, s_, t0, t1):
                j = PJB[cnt["pj"] % 3]
                cnt["pj"] += 1
                MM([(PB[j][:, 0:t1 - t0], WIN[i][:, c, s_, :], HT[:, c, t0:t1], c == 0, c == 7) for c in range(8)],
                   [("WIN", i)] + tkeys("HT", t0, t1), [PBk[j]])
                return PB[j][:, 0:t1 - t0], PBk[j]

            def sigm(out, in_, R, W):
                A(out, in_, AF.Exp, R, W, scale=-1.0)
                A(out, out, AF.Ln, W + ["ONEC"], W, bias=ONEC[:, 0:1])
                A(out, out, AF.Exp, W, W, scale=-1.0)

            def chain(h, i, d, blk, first_writer):
                (t0, t1) = blk
                n = t1 - t0
                nch = n // 64
                ch0 = t0 // 64
                is_ctx = t0 >= NLAT
                a_i, z_i, m_i = (0, 63, 31) if d == 0 else (63, 0, 32)
                k = lambda nm: (nm, d)
                cv = lambda ap: ap[:, 0:n].rearrange("p (c j) -> p c j", j=64)
                bc = lambda v: v.unsqueeze(2).to_broadcast([128, nch, 64])
                pz, pk = proj(i, 2 + d, t0, t1)
                yield
                sigm(SG[d][:, 0:n], pz, [pk], [k("SG")])
                yield
                TS("dve", KK[d][:, 0:n], SG[d][:, 0:n], -1.0, LBM1[:, h, d:d + 1], ADD, MUL, [k("SG"), "LBM1"], [k("KK")])
                A(SG[d][:, 0:n], SG[d][:, 0:n], AF.Ln, [k("SG"), "OML", "LBT"], [k("SG")], scale=OML[:, h, d:d + 1], bias=LBT[:, h, d:d + 1])
                yield
                p.op("dve", (lambda: lambda e: e.tensor_tensor_scan(out=CIN[d][:, 0:n], data0=ONES32[:, 0:n], data1=SG[d][:, 0:n],
                                                                     initial=0.0, op0=MUL, op1=ADD))(), ["ONES32", k("SG")], [k("CIN")])
                yield
                if d == 1:
                    CP("dve", TOT[d][:], CIN[d][:, n - 1:n], [k("CIN")], [k("TOT")])
                    STT("dve", D1[d][:, 0:n], CIN[d][:, 0:n], -1.0, SG[d][:, 0:n], MUL, ADD, [k("CIN"), k("SG")], [k("D1")])
                    TS("dve", CIN[d][:, 0:n], D1[d][:, 0:n], TOT[d][:, 0:1], None, ADD, None, [k("D1"), k("TOT")], [k("CIN")])
                    yield
                TT("dve", PIN[d][:, 0:nch], cv(CIN[d])[:, :, a_i], cv(SG[d])[:, :, a_i], SUB, [k("CIN"), k("SG")], [k("PIN")])
                TT("dve", DCH[d][:, 0:nch], cv(CIN[d])[:, :, z_i], PIN[d][:, 0:nch], SUB, [k("CIN"), k("PIN")], [k("DCH")])
                A(DCH[d][:, 0:nch], DCH[d][:, 0:nch], AF.Exp, [k("DCH")], [k("DCH")])
                yield
                TT("dve", cv(D1[d]), cv(CIN[d]), bc(cv(CIN[d])[:, :, z_i]), SUB, [k("CIN")], [k("D1")])
                yield
                A(D1[d][:, 0:n], D1[d][:, 0:n], AF.Exp, [k("D1")], [k("D1")], scale=-1.0)
                yield
                TT("dve", KBT[d][:, 0:n], KK[d][:, 0:n], D1[d][:, 0:n], MUL, [k("KK"), k("D1")], [k("KBT")])
                yield
                TR([(PBH[0:64, kk_ * 128:(kk_ + 1) * 128], KBT[d][:, kk_ * 64:(kk_ + 1) * 64]) for kk_ in range(nch)], IDENT[:],
                   [k("KBT"), "IDENT"], [PBHk])
                yield
                CP("act", KBCH[d][0:64, 0:nch, :], PBH[0:64, 0:nch * 128].rearrange("p (c e) -> p c e", c=nch), [PBHk], [k("KBCH")])
                yield
                MM([(PB[2 + d][:, kk_ * 128:(kk_ + 1) * 128], KBCH[d][0:64, kk_, :], VCH[0:64, ch0 + kk_, :], True, True)
                    for kk_ in range(nch)], [k("KBCH")] + tkeys("VCH", t0, t1), [PBk[2 + d]])
                yield
                if not is_ctx:
                    TT("dve", cv(D1[d]), cv(CIN[d]), bc(PIN[d][:, 0:nch]), SUB, [k("CIN"), k("PIN")], [k("D1")])
                    yield
                    A(D1[d][:, 0:n], D1[d][:, 0:n], AF.Exp, [k("D1")], [k("D1")])
                    yield
                    TT("dve", QB[d][:, 0:n], QS[:, t0:t1], D1[d][:, 0:n], MUL, tkeys("QS", t0, t1) + [k("D1")], [k("QB")])
                    TT("dve", cv(D2[d]), cv(CIN[d]), bc(cv(CIN[d])[:, :, m_i]), SUB, [k("CIN")], [k("D2")])
                    yield
                    A(SG[d][:, 0:n], D2[d][:, 0:n], AF.Exp, [k("D2")], [k("SG")], scale=-1.0)
                    A(D2[d][:, 0:n], D2[d][:, 0:n], AF.Exp, [k("D2")], [k("D2")])
                    yield
                    TT("dve", QTL[d][:, 0:n], QS[:, t0:t1], D2[d][:, 0:n], MUL, tkeys("QS", t0, t1) + [k("D2")], [k("QTL")])
                    TT("dve", KTL[d][:, 0:n], KK[d][:, 0:n], SG[d][:, 0:n], MUL, [k("KK"), k("SG")], [k("KTL")])
                    yield
                    mms = []
                    for kk_ in range(nch):
                        c0 = kk_ * 64
                        if d == 0:
                            mms.append((PB[4][0:32, c0:c0 + 64], KTL[d][:, c0:c0 + 32], QTL[d][:, c0:c0 + 64], True, True))
                            mms.append((PB[4][32:64, c0 + 32:c0 + 64], KTL[d][:, c0 + 32:c0 + 64], QTL[d][:, c0 + 32:c0 + 64], True, True))
                        else:
                            mms.append((PB[5][0:32, c0:c0 + 32], KTL[d][:, c0:c0 + 32], QTL[d][:, c0:c0 + 32], True, True))
                            mms.append((PB[5][32:64, c0:c0 + 64], KTL[d][:, c0 + 32:c0 + 64], QTL[d][:, c0:c0 + 64], True, True))
                    MM(mms, [k("KTL"), k("QTL")], [PBk[4 + d]])
                    yield
                    TT("dve", AT[d][0:64, 0:n].rearrange("p (c j) -> p c j", j=64), PB[4 + d][0:64, 0:n].rearrange("p (c j) -> p c j", j=64),
                       MASK[d][:].unsqueeze(1).to_broadcast([64, nch, 64]), MUL, [PBk[4 + d], "MASK"], [k("AT")])
                    yield
                for kk_ in (range(nch) if d == 0 else range(nch - 1, -1, -1)):
                    if not is_ctx:
                        CP("act", SBF[d][:, kk_, :], S[d][:], [k("S")], [("SBF", d, kk_)])
                    STT("dve", S[d][:], S[d][:], DCH[d][:, kk_:kk_ + 1], PB[2 + d][:, kk_ * 128:(kk_ + 1) * 128], MUL, ADD,
                        [k("S"), k("DCH"), PBk[2 + d]], [k("S")])
                    yield
                if is_ctx:
                    return
                po = PB[4 + d][:, BL:BL + n]
                pok = PBk[4 + d]
                mms = []
                for kk_ in range(nch):
                    c0 = kk_ * 64
                    mms.append((PB[4 + d][:, BL + c0:BL + c0 + 64], SBF[d][:, kk_, :], QB[d][:, c0:c0 + 64], True, False))
                    mms.append((PB[4 + d][:, BL + c0:BL + c0 + 64], VCH[0:64, ch0 + kk_, :], AT[d][0:64, c0:c0 + 64], False, True))
                MM(mms, [("SBF", d, kk_) for kk_ in range(nch)] + [k("QB"), k("AT")] + tkeys("VCH", t0, t1), [pok])
                yield
                ok = [("OT", t0 // BL)]
                if first_writer:
                    CP("act", OT[:, t0:t1], po, [pok], ok)
                    yield
                    return
                TT("dve", OT[:, t0:t1], OT[:, t0:t1], po, ADD, [pok] + ok, ok)
                yield
                pg, pk = proj(i, 4, t0, t1)
                yield
                sigm(SGG[d][:, 0:n], pg, [pk], [k("SGG")])
                TT("dve", SGG[d][:, 0:n], SGG[d][:, 0:n], pg, MUL, [k("SGG"), pk], [k("SGG")])
                yield
                A(SQ[d][:, 0:n], OT[:, t0:t1], AF.Square, ok, [k("HSQ")])
                yield
                j_ = PJB[cnt["pj"] % 3]
                cnt["pj"] += 1
                pss, pssk = PB[j_][:, 0:n], PBk[j_]
                MM([(pss, ONESB[:], SQ[d][:, 0:n], True, True)], ["ONESB", k("HSQ")], [pssk])
                yield
                A(RS[d][:, 0:n], pss, AF.Ln, [pssk, "EPSC"], [k("HRS")], scale=1.0 / 128, bias=EPSC[:, 0:1])
                A(RS[d][:, 0:n], RS[d][:, 0:n], AF.Exp, [k("HRS")], [k("HRS")], scale=-0.5)
                yield
                STT("dve", D2[d][:, 0:n], OT[:, t0:t1], GOUT[:, 0:1], RS[d][:, 0:n], MUL, MUL, ok + ["GOUT", k("HRS")], [k("D2")])
                yield
                TT("dve", ONT[i][:, t0:t1], D2[d][:, 0:n], SGG[d][:, 0:n], MUL, [k("D2"), k("SGG")], tkeys(("HONT", i), t0, t1))
                yield

            def run_interleaved(gens):
                gens = list(gens)
                while gens:
                    for g in list(gens):
                        try:
                            next(g)
                        except StopIteration:
                            gens.remove(g)

            def gen_wo(hh):
                ii = hh % 2
                for t in range(16):
                    for dbl in range(2):
                        b_ = PJB[cnt["pj"] % 3]
                        cnt["pj"] += 1
                        ds = slice(dbl * 512, (dbl + 1) * 512)
                        MM([(PB[b_][:], ONT[ii][:, t * 128:(t + 1) * 128], WOG[ii][:, ds], True, True)], [(("HONT", ii), t), ("HWOG", ii)], [PBk[b_]])
                        TT("dve", X[:, t, ds], X[:, t, ds], PB[b_][:], ADD, [("X", t), PBk[b_]], [("X", t)])
                        yield

            for h in range(8):
                i = h % 2
                if h + 1 < 8:
                    load(h + 1)
                TT("pool", WOG[i][:], WO[i][:], G_l[:], MUL, [("HWO", i), "G_l"], [("HWOG", i)])
                blks = [ctx_block] + lat_blocks

                def v_tail(idx):
                    (t0, t1) = blks[idx]
                    n = t1 - t0
                    nch = n // 64
                    ch0 = t0 // 64
                    vb = idx % 2
                    TR([(PBH[0:64, kk_ * 128:(kk_ + 1) * 128], VTF[vb][:, kk_ * 64:(kk_ + 1) * 64]) for kk_ in range(nch)], IDENT[:],
                       [("VTF", vb), "IDENT"], [PBHk])
                    CP("dve", VCH[0:64, ch0:ch0 + nch, :], PBH[0:64, 0:nch * 128].rearrange("p (c e) -> p c e", c=nch), [PBHk],
                       tkeys("VCH", t0, t1))
                for idx, (t0, t1) in enumerate(blks):
                    n = t1 - t0
                    pq, pk = proj(i, 0, t0, t1)
                    A(QS[:, t0:t1], pq, AF.Silu, [pk], tkeys("QS", t0, t1))
                    pv, pk = proj(i, 1, t0, t1)
                    CP("dve", VTF[idx % 2][:, 0:n], pv, [pk], [("VTF", idx % 2)])
                    if idx >= 1:
                        v_tail(idx - 1)
                v_tail(len(blks) - 1)
                for d in range(2):
                    MEMSET("dve", S[d][:], 0.0, [("S", d)])
                bg = [gen_wo(h - 1)] if h > 0 else []
                for s_ in range(NB + 1):
                    if s_ == 0:
                        gens = [chain(h, i, 0, ctx_block, False), chain(h, i, 1, ctx_block, False)]
                    else:
                        fw = s_ <= NB // 2
                        gens = [chain(h, i, 0, lat_blocks[s_ - 1], fw), chain(h, i, 1, lat_blocks[NB - s_], fw)]
                    run_interleaved(gens + bg)
            run_interleaved([gen_wo(7)])
            p.barrier()

    out_streamed = [False]

    def finish():
        yv = yout.rearrange("(n p) d -> p n d", p=128)
        if not out_streamed[0]:
            for g in range(0, 16, 4):
                DMA("sp", "yout", [(yv[:, g:g + 4, :], X[:, g:g + 4, :])], [("X", t) for t in range(g, g + 4)], [])
        slots = ["yout"] + (["tap"] if B.tap_outs else [])
        p.emit(final_wait_slots=slots)
        return B

    norm_phase(0, "mix", NT)
    B.tap("HT0", HT[:], [128, 8, T], [("HT", t) for t in range(NT)], BF16)
    B.tap("G_l0", G_l[:], [128, D], ["G_l"])
    if stop == "norm0":
        return finish()
    attention_phase()
    B.tap("XMIX0", X[:], [128, NT, D], [("X", t) for t in range(NT)])
    if stop == "attn0":
        return finish()
    norm_phase(0, "ffn", NT)
    moe_phase(0, NT)
    B.tap("XFFN0", X[:], [128, NT, D], [("X", t) for t in range(NT)])
    if stop == "moe0":
        return finish()
    norm_phase(1, "mix", NT)
    hgrn_phase()
    B.tap("XMIX1", X[:], [128, NT, D], [("X", t) for t in range(NT)])
    if stop == "hgrn1":
        return finish()
    norm_phase(1, "ffn", 16)
    moe_phase(1, 16)
    return finish()


def _consts():
    ident = np.eye(128, dtype=np.float32)
    blk = np.zeros((128, 128), np.float32)
    blk[:64, :64] = 1.0
    blk[64:, 64:] = 1.0
    rm = np.zeros((128, 128), np.float32)
    for fp in range(128):
        j = fp % 64
        if j < 32:
            rm[fp + 32, fp] = -1.0
        else:
            rm[fp - 32, fp] = 1.0
    t = np.arange(NLAT)
    r = (t // 64).astype(np.float32)
    col = (t % 64).astype(np.float32)
    inv = (10000.0 ** (-np.arange(16, dtype=np.float32) / 16)).astype(np.float32)
    ang = np.concatenate([r[:, None] * inv, col[:, None] * inv], axis=-1).astype(np.float32)
    cosT = np.cos(ang).astype(np.float32).T
    sinT = np.sin(ang).astype(np.float32).T
    cos = np.tile(cosT, (4, 1))
    sin = np.tile(sinT, (4, 1))
    s_idx = np.arange(64)[:, None]
    t_idx = np.arange(64)[None, :]
    maskf = (s_idx <= t_idx).astype(np.float32)
    maskb = (s_idx >= t_idx).astype(np.float32)
    return dict(cst_ident=ident, cst_blk=blk, cst_rm=rm, cst_cos=np.ascontiguousarray(cos), cst_sin=np.ascontiguousarray(sin),
                cst_maskf=maskf, cst_maskb=maskb)


def make_in_maps(inputs):
    f = lambda a: np.ascontiguousarray(np.asarray(a, dtype=np.float32))
    shared = dict(
        ada_w=f(inputs["ada_w"]), ada_b=f(inputs["ada_b"]), norm_mix=f(inputs["norm_mix"]), norm_ffn=f(inputs["norm_ffn"]),
        attn_w_qkv=f(inputs["attn_w_qkv"])[0], attn_w_o=f(inputs["attn_w_o"])[0], attn_q_norm=f(inputs["attn_q_norm"])[0],
        attn_k_norm=f(inputs["attn_k_norm"])[0], attn_sub_norm=f(inputs["attn_sub_norm"])[0],
        attn_lambda=f(inputs["attn_lambda"])[0].reshape(256), hgrn_w_in=f(inputs["hgrn_w_in"])[0],
        hgrn_w_o=f(inputs["hgrn_w_o"])[0], hgrn_out_norm=f(inputs["hgrn_out_norm"])[0],
        hgrn_lb_gamma=f(inputs["hgrn_lb_gamma"]), router_w=f(inputs["router_w"]), router_bias=f(inputs["router_bias"]),
        moe_w_gate=f(inputs["moe_w_gate"]), moe_w_up=f(inputs["moe_w_up"]), moe_w_down=f(inputs["moe_w_down"]),
    )
    shared.update(_consts())
    x = f(inputs["x"])
    ctx = f(inputs["ctx"])
    c = f(inputs["c"])
    c_ctx = f(inputs["c_ctx"])
    maps = []
    for b in range(8):
        m = dict(shared)
        m["xin"] = np.ascontiguousarray(np.concatenate([x[b], ctx[b]], axis=0))
        m["cc"] = np.ascontiguousarray(np.stack([c[b], c_ctx], axis=0))
        maps.append(m)
    return maps


def kernel(**inputs):
    B = build()
    maps = make_in_maps(inputs)
    res = run_bass_kernel_spmd(B.nc, maps, core_ids=list(range(8)))
    return np.stack([np.asarray(r["y"], dtype=np.float32) for r in res.results], axis=0)
```

```python
import contextlib
import math

import numpy as np
import concourse.bass as bass
import concourse.mybir as mybir
from concourse.bass_utils import run_bass_kernel_spmd

F32 = mybir.dt.float32
BF16 = mybir.dt.bfloat16
AF = mybir.ActivationFunctionType
ALU = mybir.AluOpType
AX = mybir.AxisListType

D = 1024
NLAT = 2048
NCTX = 256
T = NLAT + NCTX
NT = T // 128
NE = 16
DFF = 512
EPS = 1e-6
BIG = 1.0e4


class _Op:
    __slots__ = ("eng", "fn", "deps", "is_dma", "slot", "ndma", "cum", "sig", "signals", "barriered")


class Prog:
    ENG = ("pe", "act", "dve", "pool", "sp")

    def __init__(self):
        self.nc = bass.Bass("TRN2", target_bir_lowering=False)
        self.ops = []
        self.last_w = {}
        self.readers = {}
        self.slot_cnt = {}
        self.last_on = {e: None for e in self.ENG}

    def _eng(self, e):
        nc = self.nc
        return {"pe": nc.tensor, "act": nc.scalar, "dve": nc.vector, "pool": nc.gpsimd, "sp": nc.sync}[e]

    def _deps(self, reads, writes):
        deps = set()
        for k in reads:
            w = self.last_w.get(k)
            if w is not None:
                deps.add(w)
        for k in writes:
            w = self.last_w.get(k)
            if w is not None:
                deps.add(w)
            for r in self.readers.get(k, ()):
                deps.add(r)
        return deps

    def _commit(self, oid, reads, writes):
        for k in reads:
            self.readers.setdefault(k, []).append(oid)
        for k in writes:
            self.last_w[k] = oid
            self.readers[k] = []

    def op(self, eng, fn, reads=(), writes=()):
        reads = list(reads)
        writes = list(writes)
        o = _Op()
        o.eng = eng
        o.fn = fn
        o.is_dma = False
        o.deps = self._deps(reads, writes)
        if eng == "pe":
            o.deps = {d for d in o.deps if self.ops[d].is_dma or self.ops[d].eng != "pe"}
        o.signals = False
        oid = len(self.ops)
        self.ops.append(o)
        self._commit(oid, reads, writes)
        self.last_on[eng] = oid
        return oid

    def dma(self, queue, slot, fn, reads=(), writes=(), ndma=1):
        reads = list(reads)
        writes = list(writes)
        o = _Op()
        o.eng = queue
        o.fn = fn
        o.is_dma = True
        o.slot = slot
        o.ndma = ndma
        o.barriered = False
        self.slot_cnt[slot] = self.slot_cnt.get(slot, 0) + ndma
        o.cum = self.slot_cnt[slot]
        o.deps = self._deps(reads, writes)
        o.signals = False
        oid = len(self.ops)
        self.ops.append(o)
        self._commit(oid, reads, writes)
        return oid

    def barrier(self):
        pend = set()
        for e in self.ENG:
            if self.last_on[e] is not None:
                pend.add(self.last_on[e])
        for i, o in enumerate(self.ops):
            if o.is_dma and not o.barriered:
                pend.add(i)
                o.barriered = True
        for e in self.ENG:
            o = _Op()
            o.eng = e
            o.fn = None
            o.is_dma = False
            o.deps = set(pend)
            o.signals = False
            self.ops.append(o)
        self.last_w = {}
        self.readers = {}

    def emit(self, final_wait_slots=()):
        nc = self.nc
        ops = self.ops
        for o in ops:
            for d in o.deps:
                if not ops[d].is_dma:
                    ops[d].signals = True
        self._stack = contextlib.ExitStack()
        esem = {e: self._stack.enter_context(nc.semaphore("s_" + e)) for e in self.ENG}
        ssem = {s: self._stack.enter_context(nc.semaphore("d_" + str(s))) for s in self.slot_cnt}
        cnt = {e: 0 for e in self.ENG}
        for o in ops:
            if not o.is_dma and o.signals:
                cnt[o.eng] += 1
                o.sig = cnt[o.eng]
        waited = {e: {} for e in self.ENG}
        for o in ops:
            eng = self._eng(o.eng)
            w = waited[o.eng]
            need = {}
            for d in o.deps:
                od = ops[d]
                if od.is_dma:
                    key = ("d", od.slot)
                    val = 16 * od.cum
                else:
                    key = ("e", od.eng)
                    val = od.sig
                if need.get(key, 0) < val:
                    need[key] = val
            for key, val in need.items():
                if w.get(key, 0) >= val:
                    continue
                sem = ssem[key[1]] if key[0] == "d" else esem[key[1]]
                eng.wait_ge(sem, val)
                w[key] = val
            if o.fn is None:
                continue
            if o.is_dma:
                insts = o.fn(eng)
                assert len(insts) == o.ndma, (len(insts), o.ndma)
                for ins in insts:
                    ins.then_inc(ssem[o.slot], 16)
            else:
                ins = o.fn(eng)
                if o.signals:
                    ins.then_inc(esem[o.eng], 1)
        for s in final_wait_slots:
            nc.sync.wait_ge(ssem[s], 16 * self.slot_cnt[s])
        return nc


class Builder:
    def __init__(self, stop=None, taps=()):
        self.p = Prog()
        self.nc = self.p.nc
        self.stop = stop
        self.taps = set(taps)
        self.tap_outs = []
        self.uid = 0

    def din(self, name, shape, dt=F32):
        return self.nc.dram_tensor(name, list(shape), dt, kind="ExternalInput").ap()

    def dout(self, name, shape, dt=F32):
        return self.nc.dram_tensor(name, list(shape), dt, kind="ExternalOutput").ap()

    def sb(self, st, name, shape, dt=F32):
        self.uid += 1
        return st.enter_context(self.nc.sbuf_tensor(f"{name}_{self.uid}", list(shape), dt))

    def tap(self, name, src_ap, shape, keys, dt=F32):
        if name not in self.taps:
            return
        o = self.dout("tap_" + name, shape, dt)
        self.p.dma("sp", "tap", lambda e: [e.dma_start(out=o, in_=src_ap)], reads=keys)
        self.tap_outs.append("tap_" + name)

    def A(self, out, in_, func, R, W, scale=None, bias=None, accum=None):
        kw = {}
        if scale is not None:
            kw["scale"] = scale
        if bias is not None:
            kw["bias"] = bias
        if accum is not None:
            kw["accum_out"] = accum
        self.p.op("act", lambda e: e.activation(out=out, in_=in_, func=func, **kw), R, W)

    def CP(self, eng, out, in_, R, W):
        if eng == "act":
            self.p.op("act", lambda e: e.copy(out=out, in_=in_), R, W)
        else:
            self.p.op(eng, lambda e: e.tensor_copy(out=out, in_=in_), R, W)

    def TT(self, eng, out, in0, in1, op, R, W):
        self.p.op(eng, lambda e: e.tensor_tensor(out=out, in0=in0, in1=in1, op=op), R, W)

    def TS(self, eng, out, in0, s1, s2, op0, op1, R, W):
        if s2 is None:
            self.p.op(eng, lambda e: e.tensor_scalar(out=out, in0=in0, scalar1=s1, scalar2=None, op0=op0), R, W)
        else:
            self.p.op(eng, lambda e: e.tensor_scalar(out=out, in0=in0, scalar1=s1, scalar2=s2, op0=op0, op1=op1), R, W)

    def STT(self, eng, out, in0, scalar, in1, op0, op1, R, W):
        self.p.op(eng, lambda e: e.scalar_tensor_tensor(out=out, in0=in0, scalar=scalar, in1=in1, op0=op0, op1=op1), R, W)

    def RED(self, out, in_, op, R, W):
        self.p.op("dve", lambda e: e.tensor_reduce(out=out, in_=in_, axis=AX.X, op=op), R, W)

    def RECIP(self, out, in_, R, W):
        self.p.op("dve", lambda e: e.reciprocal(out=out, in_=in_), R, W)

    def MEMSET(self, eng, ap, val, W):
        self.p.op(eng, lambda e: e.memset(ap, val), (), W)

    def MM(self, mms, R, W):
        mms = list(mms)

        def fn(e):
            ins = None
            for (o, l, r, s0, s1) in mms:
                ins = e.matmul(o, lhsT=l, rhs=r, start=s0, stop=s1)
            return ins
        self.p.op("pe", fn, R, W)

    def TR(self, trs, ident, R, W):
        trs = list(trs)

        def fn(e):
            ins = None
            for (o, i) in trs:
                ins = e.transpose(out=o, in_=i, identity=ident)
            return ins
        self.p.op("pe", fn, R, W)

    def DMA(self, queue, slot, pairs, R, W, slow=False):
        pairs = list(pairs)

        def fn(e):
            if slow:
                return [e.dma_start(out=o, in_=i, allow_slow_non_contiguous=True) for (o, i) in pairs]
            return [e.dma_start(out=o, in_=i) for (o, i) in pairs]
        self.p.dma(queue, slot, fn, R, W, ndma=len(pairs))


def tkeys(name, t0, t1):
    return [(name, t) for t in range(t0 // 128, (t1 + 127) // 128)]


def build(stop=None, taps=()):
    B = Builder(stop, taps)
    p, nc = B.p, B.nc
    A, CP, TT, TS, STT, RED, RECIP, MEMSET, MM, TR, DMA = B.A, B.CP, B.TT, B.TS, B.STT, B.RED, B.RECIP, B.MEMSET, B.MM, B.TR, B.DMA
    MUL, ADD, SUB, MAX, ISEQ = ALU.mult, ALU.add, ALU.subtract, ALU.max, ALU.is_equal

    xin = B.din("xin", [T, D])
    cc = B.din("cc", [2, D])
    ada_w = B.din("ada_w", [2, D, 6 * D])
    ada_b = B.din("ada_b", [2, 6 * D])
    norm_mix = B.din("norm_mix", [2, D])
    norm_ffn = B.din("norm_ffn", [2, D])
    w_qkv = B.din("attn_w_qkv", [D, 3 * D])
    w_ao = B.din("attn_w_o", [D, D])
    q_norm = B.din("attn_q_norm", [64])
    k_norm = B.din("attn_k_norm", [64])
    sub_norm = B.din("attn_sub_norm", [128])
    a_lambda = B.din("attn_lambda", [256])
    w_in = B.din("hgrn_w_in", [D, 5 * D])
    w_ho = B.din("hgrn_w_o", [D, D])
    out_norm = B.din("hgrn_out_norm", [128])
    lb_gamma = B.din("hgrn_lb_gamma", [2, 2, D])
    router_w = B.din("router_w", [D, NE])
    router_b = B.din("router_bias", [NE])
    w_gate = B.din("moe_w_gate", [2, NE, D, DFF])
    w_up = B.din("moe_w_up", [2, NE, D, DFF])
    w_down = B.din("moe_w_down", [2, NE, DFF, D])
    c_ident = B.din("cst_ident", [128, 128])
    c_blk = B.din("cst_blk", [128, 128])
    c_rm = B.din("cst_rm", [128, 128])
    c_cos = B.din("cst_cos", [128, NLAT])
    c_sin = B.din("cst_sin", [128, NLAT])
    c_maskf = B.din("cst_maskf", [64, 64])
    c_maskb = B.din("cst_maskb", [64, 64])
    yout = B.dout("y", [NLAT, D])

    top = contextlib.ExitStack()
    sb = B.sb
    X = sb(top, "X", [128, NT, D])
    HT = sb(top, "HT", [128, 8, T], BF16)
    G_l = sb(top, "G_l", [128, D])
    G_c = sb(top, "G_c", [128, D])
    IDENT = sb(top, "IDENT", [128, 128], BF16)
    ONESB = sb(top, "ONESB", [128, 128], BF16)
    BLK = sb(top, "BLK", [128, 128], BF16)
    RM = sb(top, "RM", [128, 128], BF16)
    EPSC = sb(top, "EPSC", [128, 1])
    CRL = sb(top, "CRL", [128, 8, 128], BF16)
    CRC = sb(top, "CRC", [128, 8, 128], BF16)
    GATES = sb(top, "GATES", [128, NT, NE])
    RB = sb(top, "RB", [128, NE])
    RWH = sb(top, "RWH", [128, 8, NE], BF16)
    RWL = sb(top, "RWL", [128, 8, NE], BF16)
    PB = [top.enter_context(nc.psum_tensor(f"PB{i}", [128, 512], F32)) for i in range(6)] + [None, None]
    PBHL = [None]
    PBk = [("PB", i) for i in range(8)]
    PBHk = ("PBH",)
    flex_ctr = [0]

    def flex_psum(st, all_f32):
        flex_ctr[0] += 1
        PB[6] = st.enter_context(nc.psum_tensor(f"PB6_{flex_ctr[0]}", [128, 512], F32))
        if all_f32:
            PB[7] = st.enter_context(nc.psum_tensor(f"PB7_{flex_ctr[0]}", [128, 512], F32))
            PBHL[0] = None
        else:
            PB[7] = None
            PBHL[0] = st.enter_context(nc.psum_tensor(f"PBH_{flex_ctr[0]}", [128, 1024], BF16))

    with contextlib.ExitStack() as st:
        C32 = sb(st, "C32", [128, 3, 128])
        CCT = sb(st, "CCT", [128, 8, 2])
        SCT = sb(st, "SCT", [128, 8, 2])
        RW32 = sb(st, "RW32", [128, 8, NE])
        RWT = sb(st, "RWT", [128, 8, NE])
        DMA("sp", "cst0", [(C32[:, 0, :], c_ident), (C32[:, 1, :], c_blk), (C32[:, 2, :], c_rm)], [], ["C32"])
        DMA("sp", "cst1", [(CCT[:, :, 0], cc[0].rearrange("(c p) -> p c", p=128)),
                          (CCT[:, :, 1], cc[1].rearrange("(c p) -> p c", p=128))], [], ["CCT"], slow=True)
        DMA("sp", "cst2", [(RW32[:], router_w.rearrange("(c p) e -> p c e", p=128)),
                          (RB[:], router_b.partition_broadcast(128))], [], ["RW32", "RB"])
        CP("dve", IDENT[:], C32[:, 0, :], ["C32"], ["IDENT"])
        CP("dve", BLK[:], C32[:, 1, :], ["C32"], ["BLK"])
        CP("dve", RM[:], C32[:, 2, :], ["C32"], ["RM"])
        MEMSET("dve", ONESB[:], 1.0, ["ONESB"])
        MEMSET("dve", EPSC[:], EPS, ["EPSC"])
        A(SCT[:], CCT[:], AF.Silu, ["CCT"], ["SCT"])
        CP("dve", CRL[:], SCT[:, :, 0:1].to_broadcast([128, 8, 128]), ["SCT"], ["CRL"])
        CP("dve", CRC[:], SCT[:, :, 1:2].to_broadcast([128, 8, 128]), ["SCT"], ["CRC"])
        CP("dve", RWH[:], RW32[:], ["RW32"], ["RWH"])
        TT("dve", RWT[:], RW32[:], RWH[:], SUB, ["RW32", "RWH"], ["RWT"])
        CP("dve", RWL[:], RWT[:], ["RWT"], ["RWL"])
        xv = xin.rearrange("(n p) d -> p n d", p=128)
        for g in range(0, NT, 3):
            DMA("sp", f"xld{g}", [(X[:, g:g + 3, :], xv[:, g:g + 3, :])], [], [("X", t) for t in range(g, g + 3)])
        p.barrier()

    def modslice(ADAW, BIAS, layer, j, out_l, out_c, defer=False):
        DMA("sp", "adab", [(BIAS[:], ada_b[layer, j * D:(j + 1) * D].partition_broadcast(128))], [], ["BIAS"])
        for half in range(2):
            cs_ = slice(j * D + half * 512, j * D + (half + 1) * 512)
            DMA("pool", f"adaw{half}", [(ADAW[half][:], ada_w[layer, :, cs_].rearrange("(c p) n -> p c n", p=128))], [], [("ADAW", half)])
        def compute():
            k = 0
            for half in range(2):
                for CR, crk, out in ((CRL, "CRL", out_l), (CRC, "CRC", out_c)):
                    if out is None:
                        continue
                    bank = k % 2
                    k += 1
                    MM([(PB[bank][:], CR[:, c, :], ADAW[half][:, c, :], c == 0, c == 7) for c in range(8)],
                       [crk, ("ADAW", half)], [PBk[bank]])
                    TT("dve", out[0][:, half * 512:(half + 1) * 512], PB[bank][:], BIAS[:, half * 512:(half + 1) * 512], ADD,
                       [PBk[bank], "BIAS"], [(out[1], half)])
        if defer:
            return compute
        compute()
        return None

    def norm_phase(layer, which, ntiles):
        j0 = 0 if which == "mix" else 3
        nw = norm_mix if which == "mix" else norm_ffn
        with_ctx = ntiles > 16
        with contextlib.ExitStack() as st:
            flex_psum(st, False)
            PBH = PBHL[0]
            ADAW = [sb(st, f"ADAW{i}", [128, 8, 512], BF16) for i in range(2)]
            BIAS = sb(st, "BIAS", [128, D])
            GN = sb(st, "GN", [128, D])
            SC_l = sb(st, "SC_l", [128, D])
            SC_c = sb(st, "SC_c", [128, D])
            A_l = sb(st, "A_l", [128, D])
            A_c = sb(st, "A_c", [128, D])
            SH_l = sb(st, "SH_l", [128, D])
            SH_c = sb(st, "SH_c", [128, D])
            TMP = [sb(st, f"TMP{i}", [128, D]) for i in range(2)]
            HB = [sb(st, f"HB{i}", [128, D], BF16) for i in range(2)]
            JUNK = HB[1]
            SS = sb(st, "SS", [128, NT])
            RSTD = sb(st, "RSTD", [128, NT])
            if which == "ffn":
                H32 = [sb(st, f"H32{i}", [128, D]) for i in range(2)]
                HLO = [sb(st, f"HLO{i}", [128, D], BF16) for i in range(2)]
                HTLO = [sb(st, f"HTLO{i}", [128, 8, 128], BF16) for i in range(2)]
                LG = sb(st, "LG", [128, NT, NE])
                RT = {n: sb(st, f"RT_{n}", [128, NT, NE]) for n in ("AFF", "BI", "EQ", "MB", "E1")}
                RS = {n: sb(st, f"RS_{n}", [128, NT, 4]) for n in ("M1", "M2", "GS", "PEN")}
                RV = {n: sb(st, f"RV_{n}", [128, NT]) for n in ("GM", "V1", "V2", "SUM")}
            DMA("sp", "gn", [(GN[:], nw[layer].partition_broadcast(128))], [], ["GN"])
            modslice(ADAW, BIAS, layer, j0 + 0, (SH_l, "SH_l"), (SH_c, "SH_c") if with_ctx else None)
            modslice(ADAW, BIAS, layer, j0 + 1, (SC_l, "SC_l"), (SC_c, "SC_c") if with_ctx else None)
            STT("dve", A_l[:], SC_l[:], 1.0, GN[:], ADD, MUL, [("SC_l", 0), ("SC_l", 1), "GN"], ["A_l"])
            if with_ctx:
                STT("dve", A_c[:], SC_c[:], 1.0, GN[:], ADD, MUL, [("SC_c", 0), ("SC_c", 1), "GN"], ["A_c"])
            gate_compute = modslice(ADAW, BIAS, layer, j0 + 2, (G_l, "G_l"), (G_c, "G_c") if with_ctx else None, defer=True)
            MEMSET("dve", SS[:], 0.0, ["SS"])
            for t in range(ntiles):
                A(JUNK[:], X[:, t, :], AF.Square, [("X", t), "SS"], [("HB", 1), "SS"], accum=SS[:, t:t + 1])
            A(RSTD[:, 0:ntiles], SS[:, 0:ntiles], AF.Sqrt, ["SS", "EPSC"], ["RSTD"], scale=1.0 / D, bias=EPSC[:, 0:1])
            RECIP(RSTD[:, 0:ntiles], RSTD[:, 0:ntiles], ["RSTD"], ["RSTD"])
            def stage_a(t):
                i = t % 2
                lat = t < 16
                Aw, Ak = (A_l, "A_l") if lat else (A_c, "A_c")
                Sw, Sk = (SH_l, "SH_l") if lat else (SH_c, "SH_c")
                Sks = [(Sk, 0), (Sk, 1)]
                STT("dve", TMP[i][:], X[:, t, :], RSTD[:, t:t + 1], Aw[:], MUL, MUL, [("X", t), "RSTD", Ak], [("TMP", i)])
                if which == "ffn" and t >= 1:
                    stage_b(t - 1)
                if which == "mix":
                    TT("pool" if t % 2 == 0 else "dve", HB[i][:], TMP[i][:], Sw[:], ADD, [("TMP", i)] + Sks, [("HB", i)])
                else:
                    TT("pool", H32[i][:], TMP[i][:], Sw[:], ADD, [("TMP", i)] + Sks, [("H32", i)])
                    CP("act", HB[i][:], H32[i][:], [("H32", i)], [("HB", i)])
                    TT("dve", HLO[i][:], H32[i][:], HB[i][:], SUB, [("H32", i), ("HB", i)], [("HLO", i)])
                TR([(PBH[:, c * 128:(c + 1) * 128], HB[i][:, c * 128:(c + 1) * 128]) for c in range(8)], IDENT[:],
                   [("HB", i), "IDENT"], [PBHk])
                CP("act", HT[:, :, t * 128:(t + 1) * 128], PBH[:].rearrange("p (c j) -> p c j", c=8), [PBHk], [("HT", t)])
                if which == "ffn":
                    TR([(PBH[:, c * 128:(c + 1) * 128], HLO[i][:, c * 128:(c + 1) * 128]) for c in range(8)], IDENT[:],
                       [("HLO", i), "IDENT"], [PBHk])
                    CP("act", HTLO[i][:], PBH[:].rearrange("p (c j) -> p c j", c=8), [PBHk], [("HTLO", i)])

            def stage_b(t):
                i = t % 2
                lg = PB[2 + i][:, 0:NE]
                mms = []
                for c in range(8):
                    mms.append((lg, HT[:, c, t * 128:(t + 1) * 128], RWH[:, c, :], c == 0, False))
                    mms.append((lg, HTLO[i][:, c, :], RWH[:, c, :], False, False))
                    mms.append((lg, HT[:, c, t * 128:(t + 1) * 128], RWL[:, c, :], False, c == 7))
                MM(mms, [("HT", t), ("HTLO", i), "RWH", "RWL"], [PBk[2 + i]])
                CP("act", LG[:, t, :], lg, [PBk[2 + i]], [("LG", t)])

            def router_all(nt):
                f3 = lambda ap: ap[:, 0:nt, :]
                v4 = lambda ap: ap[:, 0:nt, :].rearrange("p t (g j) -> p t g j", g=4)
                g3 = lambda ap: ap[:, 0:nt, :]
                v2 = lambda ap: ap[:, 0:nt]
                bj = lambda ap: ap[:, 0:nt, :].unsqueeze(3).to_broadcast([128, nt, 4, 4])
                be = lambda ap: ap[:, 0:nt].unsqueeze(2).to_broadcast([128, nt, NE])
                bg4 = lambda ap: ap[:, 0:nt].unsqueeze(2).to_broadcast([128, nt, 4])
                AFF, BI, EQ, MB, E1 = (RT[n] for n in ("AFF", "BI", "EQ", "MB", "E1"))
                E2 = EQ
                M1, M2, GS, PEN = (RS[n] for n in ("M1", "M2", "GS", "PEN"))
                GM, V1, V2, SUM = (RV[n] for n in ("GM", "V1", "V2", "SUM"))
                A(f3(AFF), f3(LG), AF.Sigmoid, [("LG", t) for t in range(nt)], ["AFF"])
                TT("dve", f3(BI), f3(AFF), RB[:].unsqueeze(1).to_broadcast([128, nt, NE]), ADD, ["AFF", "RB"], ["BI"])
                RED(g3(M1), v4(BI), MAX, ["BI"], ["M1"])
                TT("dve", v4(EQ), v4(BI), bj(M1), ISEQ, ["BI", "M1"], ["EQ"])
                STT("dve", f3(EQ), f3(EQ), -BIG, f3(BI), MUL, ADD, ["EQ", "BI"], ["EQ"])
                RED(g3(M2), v4(EQ), MAX, ["EQ"], ["M2"])
                TT("dve", g3(GS), g3(M1), g3(M2), ADD, ["M1", "M2"], ["GS"])
                RED(v2(GM), g3(GS), MAX, ["GS"], ["GM"])
                TT("dve", g3(M1), g3(GS), bg4(GM), ISEQ, ["GS", "GM"], ["M1"])
                TS("dve", g3(PEN), g3(M1), -1.0, BIG, ADD, MUL, ["M1"], ["PEN"])
                TT("dve", v4(MB), v4(BI), bj(PEN), ADD, ["BI", "PEN"], ["MB"])
                RED(v2(V1), f3(MB), MAX, ["MB"], ["V1"])
                TT("dve", f3(E1), f3(MB), be(V1), ISEQ, ["MB", "V1"], ["E1"])
                STT("dve", f3(MB), f3(E1), -BIG, f3(MB), MUL, ADD, ["E1", "MB"], ["MB"])
                RED(v2(V2), f3(MB), MAX, ["MB"], ["V2"])
                TT("dve", f3(E2), f3(MB), be(V2), ISEQ, ["MB", "V2"], ["EQ"])
                TT("dve", f3(E1), f3(E1), f3(E2), ADD, ["E1", "EQ"], ["E1"])
                TT("dve", f3(E1), f3(AFF), f3(E1), MUL, ["AFF", "E1"], ["E1"])
                RED(v2(SUM), f3(E1), ADD, ["E1"], ["SUM"])
                RECIP(v2(SUM), v2(SUM), ["SUM"], ["SUM"])
                TT("dve", GATES[:, 0:nt, :], f3(E1), be(SUM), MUL, ["E1", "SUM"], [("GATES", t) for t in range(nt)])

            for t in range(ntiles):
                stage_a(t)
            gate_compute()
            if which == "ffn":
                stage_b(ntiles - 1)
                router_all(ntiles)
            p.barrier()


    def attention_phase():
        blocks = [(0, 512), (512, 1024), (1024, 1536), (1536, 2048), (2048, 2304)]
        with contextlib.ExitStack() as st:
            flex_psum(st, True)
            CS = [sb(st, f"CS{i}", [128, 2, 512]) for i in range(1)]
            WQKV = sb(st, "WQKV", [128, 8, 3, 128], BF16)
            WO = [sb(st, f"WO{i}", [128, D], BF16) for i in range(2)]
            WOG = [[sb(st, f"WOG{i}{g}", [128, D], BF16) for g in range(2)] for i in range(2)]
            QT = [sb(st, f"QT{i}", [128, T], BF16) for i in range(2)]
            KT = [sb(st, f"KT{i}", [128, T], BF16) for i in range(2)]
            VT = [sb(st, f"VT{i}", [128, NT, 128], BF16) for i in range(2)]
            ONT = sb(st, "ONT", [128, T], BF16)
            GQ = sb(st, "GQ", [128, 1])
            GK = sb(st, "GK", [128, 1])
            GSUB = sb(st, "GSUB", [128, 1])
            LAMB = sb(st, "LAMB", [128, 256])
            LTMP = sb(st, "LTMP", [128, 128])
            LS = sb(st, "LS", [128, 2])
            NLAM = sb(st, "NLAM", [128, 1])
            SQ = sb(st, "SQ", [128, 512], BF16)
            RST = sb(st, "RST", [128, 512])
            QN = sb(st, "QN", [128, 512], BF16)
            T1 = sb(st, "T1", [128, 512])
            T2 = sb(st, "T2", [128, 512])
            E = [sb(st, f"E{i}", [128, 512], BF16) for i in range(4)]
            ZS = [[sb(st, f"ZS{b}{i}", [128, 512]) for i in range(2)] for b in range(2)]
            OS = [[sb(st, f"OS{b}{i}", [128, 512]) for i in range(2)] for b in range(2)]

            col = lambda v: v.rearrange("(p o) -> p o", o=1)
            DMA("sp", "acst0", [(GQ[0:64, :], col(q_norm)), (GQ[64:128, :], col(q_norm)), (GK[0:64, :], col(k_norm)),
                                (GK[64:128, :], col(k_norm)), (GSUB[:], col(sub_norm)),
                                (LAMB[:], a_lambda.partition_broadcast(128))], [], ["GQ", "GK", "GSUB", "LAMB"])
            lam_init0 = 0.8 - 0.6 * math.exp(0.0)
            TS("dve", GSUB[:], GSUB[:], 1.0 - lam_init0, None, MUL, None, ["GSUB"], ["GSUB"])
            lv = LAMB[:].rearrange("p (a b d) -> p a b d", a=2, b=2)
            TT("dve", LTMP[:].rearrange("p (a d) -> p a d", a=2), lv[:, :, 0, :], lv[:, :, 1, :], MUL, ["LAMB"], ["LTMP"])
            RED(LS[:], LTMP[:].rearrange("p (a d) -> p a d", a=2), ADD, ["LTMP"], ["LS"])
            A(LS[:], LS[:], AF.Exp, ["LS"], ["LS"])
            TT("dve", NLAM[:], LS[:, 1:2], LS[:, 0:1], SUB, ["LS"], ["NLAM"])
            TS("dve", NLAM[:], NLAM[:], -lam_init0, None, ADD, None, ["NLAM"], ["NLAM"])

            cnt = {"e": 0, "cs": 0}
            qdone = {}

            def gen_proj(h):
                par = h % 2
                DMA("pool", "wqkv", [(WQKV[:, :, s_, :], w_qkv[:, s_ * D + h * 128:s_ * D + (h + 1) * 128]
                                      .rearrange("(c p) f -> p c f", p=128)) for s_ in range(3)], [], ["WQKV"])
                DMA("pool", f"wo{par}", [(WO[par][:], w_ao[h * 128:(h + 1) * 128, :])], [], [("WO", par)])
                yield

                def qk(s_, dst, gcol, gk, dkey, t0, t1):
                    n = t1 - t0
                    pq = PB[2][:, 0:n]
                    MM([(pq, WQKV[:, c, s_, :], HT[:, c, t0:t1], c == 0, c == 7) for c in range(8)],
                       ["WQKV"] + tkeys("HT", t0, t1), [PBk[2]])
                    yield
                    A(SQ[:, 0:n], pq, AF.Square, [PBk[2]], ["SQ"])
                    yield
                    yield
                    MM([(PB[3][:, 0:n], BLK[:], SQ[:, 0:n], True, True)], ["BLK", "SQ"], [PBk[3]])
                    A(RST[:, 0:n], PB[3][:, 0:n], AF.Ln, [PBk[3], "EPSC"], ["RST"], scale=1.0 / 64, bias=EPSC[:, 0:1])
                    A(RST[:, 0:n], RST[:, 0:n], AF.Exp, ["RST"], ["RST"], scale=-0.5)
                    yield
                    if t0 < NLAT:
                        cs = 0
                        DMA("sp", f"cs{cs}", [(CS[cs][:, 0, 0:n], c_cos[:, t0:t1]), (CS[cs][:, 1, 0:n], c_sin[:, t0:t1])], [], [("CS", cs)])
                        STT("dve", QN[:, 0:n], pq, gcol[:, 0:1], RST[:, 0:n], MUL, MUL, [PBk[2], gk, "RST"], ["QN"])
                        yield
                        yield
                        MM([(PB[3][:, 0:n], RM[:], QN[:, 0:n], True, True)], ["RM", "QN"], [PBk[3]])
                        TT("pool", T1[:, 0:n], QN[:, 0:n], CS[cs][:, 0, 0:n], MUL, ["QN", ("CS", cs)], ["T1"])
                        yield
                        TT("dve", T2[:, 0:n], PB[3][:, 0:n], CS[cs][:, 1, 0:n], MUL, [PBk[3], ("CS", cs)], ["T2"])
                        TT("pool", dst[:, t0:t1], T1[:, 0:n], T2[:, 0:n], ADD, ["T1", "T2"], tkeys(dkey, t0, t1))
                        yield True
                    else:
                        STT("dve", dst[:, t0:t1], pq, gcol[:, 0:1], RST[:, 0:n], MUL, MUL, [PBk[2], gk, "RST"], tkeys(dkey, t0, t1))
                        yield True
                for (t0, t1) in blocks:
                    yield from qk(1, KT[par], GK, "GK", ("KT", par), t0, t1)
                for g in range(0, NT, 4):
                    ng = min(4, NT - g)
                    mms = []
                    for k_ in range(ng):
                        tt = g + k_
                        mms += [(PB[2][:, k_ * 128:(k_ + 1) * 128], HT[:, c, tt * 128:(tt + 1) * 128], WQKV[:, c, 2, :], c == 0, c == 7)
                                for c in range(8)]
                    MM(mms, ["WQKV"] + [("HT", g + k_) for k_ in range(ng)], [PBk[2]])
                    yield
                    CP("dve", VT[par][:, g:g + ng, :], PB[2][:, 0:ng * 128].rearrange("p (a e) -> p a e", a=ng), [PBk[2]],
                       [(("VT", par), g + k_) for k_ in range(ng)])
                    yield True
                TT("pool", WOG[par][0][:], WO[par][:], G_l[:], MUL, [("WO", par), "G_l"], [("WOG", par, 0)])
                yield True
                TT("pool", WOG[par][1][:], WO[par][:], G_c[:], MUL, [("WO", par), "G_c"], [("WOG", par, 1)])
                yield True
                for bi, (t0, t1) in enumerate(blocks):
                    yield from qk(0, QT[par], GQ, "GQ", ("QT", par), t0, t1)

            def gen_wo(h):
                par = h % 2
                for t in range(NT):
                    gi = 0 if t < 16 else 1
                    for dbl in range(2):
                        ds = slice(dbl * 512, (dbl + 1) * 512)
                        b_ = 2 + dbl
                        MM([(PB[b_][:], ONT[:, t * 128:(t + 1) * 128], WOG[par][gi][:, ds], True, True)], [("ONT", t), ("WOG", par, gi)], [PBk[b_]])
                        TT("dve", X[:, t, ds], X[:, t, ds], PB[b_][:], ADD, [("X", t), PBk[b_]], [("X", t)])
                        yield True

            def gen_attn(h):
                par = h % 2
                for bi, (q0, q1) in enumerate(blocks):
                    nq = q1 - q0
                    ktiles = list(range(NT)) if q0 < NLAT else [16, 17]
                    pend = []

                    def av(ki, kt, ebs):
                        first, last = ki == 0, ki == len(ktiles) - 1
                        MM([(PB[4][:, 0:nq], VT[par][:, kt, :], E[ebs[0]][:, 0:nq], first, last),
                            (PB[5][:, 0:nq], VT[par][:, kt, :], E[ebs[1]][:, 0:nq], first, last),
                            (PB[6][:, 0:nq], ONESB[:], E[ebs[0]][:, 0:nq], first, last),
                            (PB[7][:, 0:nq], ONESB[:], E[ebs[1]][:, 0:nq], first, last)],
                           [(("VT", par), kt), ("E", ebs[0]), ("E", ebs[1]), "ONESB"], [PBk[4], PBk[5], PBk[6], PBk[7]])
                    for ki, kt in enumerate(ktiles):
                        ebs = (cnt["e"] % 4, (cnt["e"] + 1) % 4)
                        cnt["e"] += 2
                        ks = slice(kt * 128, (kt + 1) * 128)
                        for m in range(2):
                            MM([(PB[m][:, 0:nq], KT[par][m * 64:(m + 1) * 64, ks], QT[par][m * 64:(m + 1) * 64, q0:q1], True, True)],
                               [(("KT", par), kt)] + tkeys(("QT", par), q0, q1), [PBk[m]])
                            A(E[ebs[m]][:, 0:nq], PB[m][:, 0:nq], AF.Exp, [PBk[m]], [("E", ebs[m])], scale=0.125)
                        if pend:
                            av(*pend.pop())
                        pend.append((ki, kt, ebs))
                        yield
                    qdone[h] = bi + 1
                    av(*pend.pop())
                    yield
                    zb = bi % 2
                    for m in range(2):
                        CP("dve", ZS[zb][m][:, 0:nq], PB[6 + m][:, 0:nq], [PBk[6 + m]], [("ZS", zb, m)])
                        CP("act", OS[zb][m][:, 0:nq], PB[4 + m][:, 0:nq], [PBk[4 + m]], [("OS", zb, m)])
                    prio.append(gen_epi(zb, q0, q1))
                    yield

            def gen_epi(zb, q0, q1):
                nq = q1 - q0
                Z, O = ZS[zb], OS[zb]
                for m in range(2):
                    A(Z[m][:, 0:nq], Z[m][:, 0:nq], AF.Ln, [("ZS", zb, m)], [("ZS", zb, m)])
                    A(Z[m][:, 0:nq], Z[m][:, 0:nq], AF.Exp, [("ZS", zb, m)], [("ZS", zb, m)], scale=-1.0)
                    yield
                    TT("pool", O[m][:, 0:nq], O[m][:, 0:nq], Z[m][:, 0:nq], MUL, [("OS", zb, m), ("ZS", zb, m)], [("OS", zb, m)])
                    yield
                STT("dve", O[0][:, 0:nq], O[1][:, 0:nq], NLAM[:, 0:1], O[0][:, 0:nq], MUL, ADD, [("OS", zb, 0), ("OS", zb, 1), "NLAM"], [("OS", zb, 0)])
                yield
                A(SQ[:, 0:nq], O[0][:, 0:nq], AF.Square, [("OS", zb, 0)], ["SQ"])
                yield
                yield
                MM([(PB[3][:, 0:nq], ONESB[:], SQ[:, 0:nq], True, True)], ["ONESB", "SQ"], [PBk[3]])
                A(RST[:, 0:nq], PB[3][:, 0:nq], AF.Ln, [PBk[3], "EPSC"], ["RST"], scale=1.0 / 128, bias=EPSC[:, 0:1])
                A(RST[:, 0:nq], RST[:, 0:nq], AF.Exp, ["RST"], ["RST"], scale=-0.5)
                yield
                STT("dve", ONT[:, q0:q1], O[0][:, 0:nq], GSUB[:, 0:1], RST[:, 0:nq], MUL, MUL, [("OS", zb, 0), "GSUB", "RST"], tkeys("ONT", q0, q1))
                yield

            bgq = []
            prio = []
            BG_PER_MAIN = 2

            rs_state = {"cur": None, "cur_prio": False}

            def run_stage(main, drain_all=False):
                main_alive = main is not None
                cur, cur_prio = rs_state["cur"], rs_state["cur_prio"]
                while main_alive or bgq or (cur is not None and not cur_prio) or (drain_all and (prio or cur is not None)):
                    if main_alive:
                        try:
                            next(main)
                        except StopIteration:
                            main_alive = False
                    for _rep in range(BG_PER_MAIN):
                        if cur is None:
                            if prio:
                                cur, cur_prio = prio.pop(0), True
                            elif bgq:
                                cur, cur_prio = bgq[0], False
                        if cur is not None:
                            try:
                                r = next(cur)
                            except StopIteration:
                                if not cur_prio:
                                    bgq.pop(0)
                                cur = None
                                continue
                            if r is True and not cur_prio and prio:
                                cur = None
                rs_state["cur"], rs_state["cur_prio"] = cur, cur_prio

            bgq.append(gen_proj(0))
            run_stage(None)
            for h in range(8):
                if h > 0:
                    bgq.append(gen_wo(h - 1))
                if h + 1 < 8:
                    bgq.append(gen_proj(h + 1))
                run_stage(gen_attn(h))
            bgq.append(gen_wo(7))
            run_stage(None, drain_all=True)
            p.barrier()

    def moe_phase(layer, ntiles):
        blocks = [(0, 512), (512, 1024), (1024, 1536), (1536, 2048)] + ([(2048, 2304)] if ntiles > 16 else [])
        with contextlib.ExitStack() as st:
            flex_psum(st, False)
            WG = [sb(st, f"WG{i}", [128, 8, DFF], BF16) for i in range(2)]
            WU = [sb(st, f"WU{i}", [128, 8, DFF], BF16) for i in range(2)]
            WD = [sb(st, f"WD{i}", [128, 4, D], BF16) for i in range(2)]
            SG = [sb(st, f"SG{i}", [128, 512]) for i in range(2)]
            HID = [sb(st, f"HID{i}", [128, 4, 512], BF16) for i in range(2)]
            WT = [sb(st, f"WT{i}", [128, 512]) for i in range(2)]

            def load(e):
                i = e % 2
                gk = [("WG", i, f_) for f_ in range(4)]
                uk = [("WU", i, f_) for f_ in range(4)]
                if e == 0:
                    for f_ in range(4):
                        fs_ = slice(f_ * 128, (f_ + 1) * 128)
                        DMA("pool", f"wg0c{f_}", [(WG[i][:, :, fs_], w_gate[layer, e][:, fs_].rearrange("(c p) f -> p c f", p=128))], [], [gk[f_]])
                        DMA("pool", f"wu0c{f_}", [(WU[i][:, :, fs_], w_up[layer, e][:, fs_].rearrange("(c p) f -> p c f", p=128))], [], [uk[f_]])
                else:
                    DMA("pool", f"wg{i}", [(WG[i][:], w_gate[layer, e].rearrange("(c p) f -> p c f", p=128))], [], gk)
                    DMA("pool", f"wu{i}", [(WU[i][:], w_up[layer, e].rearrange("(c p) f -> p c f", p=128))], [], uk)
                DMA("pool", f"wd{i}", [(WD[i][:], w_down[layer, e].rearrange("(c p) n -> p c n", p=128))], [], [("WD", i)])
            load(0)
            cnt = {"c2": 0, "c3": 0}

            def gate_up(e, blk, hb):
                i = e % 2
                (t0, t1) = blk
                n = t1 - t0
                for ffc in range(4):
                    j = cnt["c2"] % 2
                    cnt["c2"] += 1
                    fs = slice(ffc * 128, (ffc + 1) * 128)
                    MM([(PB[j][:, 0:n], WG[i][:, c, fs], HT[:, c, t0:t1], c == 0, c == 7) for c in range(8)],
                       [("WG", i, ffc)] + tkeys("HT", t0, t1), [PBk[j]])
                    MM([(PB[2 + j][:, 0:n], WU[i][:, c, fs], HT[:, c, t0:t1], c == 0, c == 7) for c in range(8)],
                       [("WU", i, ffc)] + tkeys("HT", t0, t1), [PBk[2 + j]])
                    A(SG[j][:, 0:n], PB[j][:, 0:n], AF.Silu, [PBk[j]], [("SG", j)])
                    TT("dve", HID[hb][:, ffc, 0:n], SG[j][:, 0:n], PB[2 + j][:, 0:n], MUL, [("SG", j), PBk[2 + j]], [("HID", hb)])

            def down(e, blk, hb):
                i = e % 2
                (t0, t1) = blk
                for tt in range(t0 // 128, t1 // 128):
                    Gw, Gk_ = (G_l, "G_l") if tt < 16 else (G_c, "G_c")
                    o0 = tt * 128 - t0
                    for dbl in range(2):
                        b_ = 4 + (cnt["c3"] % 3)
                        w_ = cnt["c3"] % 2
                        cnt["c3"] += 1
                        ds = slice(dbl * 512, (dbl + 1) * 512)
                        MM([(PB[b_][:], HID[hb][:, ffc, o0:o0 + 128], WD[i][:, ffc, ds], ffc == 0, ffc == 3) for ffc in range(4)],
                           [("HID", hb), ("WD", i)], [PBk[b_]])
                        STT("dve", WT[w_][:], PB[b_][:], GATES[:, tt, e:e + 1], Gw[:, ds], MUL, MUL,
                            [PBk[b_], ("GATES", tt), Gk_], [("WT", w_)])
                        TT("pool", X[:, tt, ds], X[:, tt, ds], WT[w_][:], ADD, [("X", tt), ("WT", w_)], [("X", tt)])
                    if layer == 1 and e == NE - 1 and tt < 16:
                        DMA("sp", "yout", [(yout.rearrange("(n p) d -> p n d", p=128)[:, tt:tt + 1, :], X[:, tt:tt + 1, :])], [("X", tt)], [])
                        out_streamed[0] = True

            items = [(e, blk) for e in range(NE) for blk in blocks]
            for k_, (e, blk) in enumerate(items):
                gate_up(e, blk, k_ % 2)
                if k_ >= 1:
                    pe_, pblk = items[k_ - 1]
                    down(pe_, pblk, (k_ - 1) % 2)
                if blk == blocks[0] and e + 1 < NE:
                    load(e + 1)
            pe_, pblk = items[-1]
            down(pe_, pblk, (len(items) - 1) % 2)
            p.barrier()

    def hgrn_phase():
        BL = 256
        NCH = BL // 64
        lat_blocks = [(b * BL, (b + 1) * BL) for b in range(NLAT // BL)]
        ctx_block = (NLAT, T)
        NB = len(lat_blocks)
        with contextlib.ExitStack() as st:
            flex_psum(st, False)
            PBH = PBHL[0]
            WIN = [sb(st, f"WIN{i}", [128, 8, 5, 128], BF16) for i in range(2)]
            WO = [sb(st, f"HWO{i}", [128, D], BF16) for i in range(2)]
            WOG = [sb(st, f"HWOG{i}", [128, D], BF16) for i in range(2)]
            GAM = sb(st, "GAM", [128, 8, 2, 2])
            LBT = sb(st, "LBT", [128, 8, 2])
            OML = sb(st, "OML", [128, 8, 2])
            LBM1 = sb(st, "LBM1", [128, 8, 2])
            GOUT = sb(st, "GOUT", [128, 1])
            ONEC = sb(st, "ONEC", [128, 1])
            MASK = [sb(st, f"MASK{i}", [64, 64]) for i in range(2)]
            ONES32 = sb(st, "ONES32", [128, BL])
            QS = sb(st, "QS", [128, T])
            VCH = sb(st, "VCH", [64, T // 64, 128], BF16)
            OT = X[:, 16:18, :].rearrange("p a d -> p (a d)")
            ONT = [sb(st, f"HONT{i}", [128, NLAT], BF16) for i in range(2)]
            VTF = [sb(st, f"VTF{i}", [128, BL], BF16) for i in range(2)]
            S = [sb(st, f"S{i}", [128, 128]) for i in range(2)]
            tmp = lambda nm, shape, dt=F32: [sb(st, f"{nm}{i}", shape, dt) for i in range(2)]
            SBF = tmp("SBF", [128, NCH, 128], BF16)
            SG = tmp("HSG", [128, BL])
            KK = tmp("KK", [128, BL])
            CIN = tmp("CIN", [128, BL])
            D1 = tmp("D1", [128, BL])
            D2 = tmp("D2", [128, BL])
            QB = tmp("QB", [128, BL], BF16)
            QTL = tmp("QTL", [128, BL], BF16)
            KTL = tmp("KTL", [128, BL], BF16)
            KBT = tmp("KBT", [128, BL], BF16)
            KBCH = tmp("KBCH", [64, NCH, 128], BF16)
            AT = tmp("AT", [64, BL], BF16)
            PIN = tmp("PIN", [128, NCH])
            DCH = tmp("DCH", [128, NCH])
            TOT = tmp("TOT", [128, 1])
            SGG = tmp("SGG", [128, BL])
            SQ = tmp("HSQ", [128, BL], BF16)
            RS = tmp("HRS", [128, BL])

            for a_ in range(2):
                for l_ in range(2):
                    DMA("sp", f"hcst{a_}{l_}", [(GAM[:, :, a_, l_], lb_gamma[a_, l_].rearrange("(h p) -> p h", p=128))], [], ["GAM"], slow=True)
            DMA("sp", "hcstm", [(GOUT[:], out_norm.rearrange("(p o) -> p o", o=1)), (MASK[0][:], c_maskf), (MASK[1][:], c_maskb)],
                [], ["GOUT", "MASK"])
            TT("dve", LBT[:], GAM[:, :, :, 1], GAM[:, :, :, 0], SUB, ["GAM"], ["LBT"])
            MEMSET("dve", ONEC[:], 1.0, ["ONEC"])
            A(LBT[:], LBT[:], AF.Exp, ["LBT"], ["LBT"], scale=-1.0)
            A(LBT[:], LBT[:], AF.Ln, ["LBT", "ONEC"], ["LBT"], bias=ONEC[:, 0:1])
            A(LBT[:], LBT[:], AF.Exp, ["LBT"], ["LBT"], scale=-1.0)
            TS("dve", OML[:], LBT[:], -1.0, 1.0, MUL, ADD, ["LBT"], ["OML"])
            TS("dve", LBM1[:], LBT[:], -1.0, None, ADD, None, ["LBT"], ["LBM1"])
            MEMSET("dve", ONES32[:], 1.0, ["ONES32"])
            MEMSET("dve", PB[4][:], 0.0, [PBk[4]])
            MEMSET("dve", PB[5][:], 0.0, [PBk[5]])

            def load(h):
                i = h % 2
                DMA("pool", f"win{i}", [(WIN[i][:, :, s_, :], w_in[:, s_ * D + h * 128:s_ * D + (h + 1) * 128]
                                         .rearrange("(c p) f -> p c f", p=128)) for s_ in range(5)], [], [("WIN", i)])
                DMA("pool", f"hwo{i}", [(WO[i][:], w_ho[h * 128:(h + 1) * 128, :])], [], [("HWO", i)])
            load(0)
            cnt = {"pj": 0, "w": 0}

            PJB = [0, 1, 6]

            def proj(i, s_, t0, t1):
                j = PJB[cnt["pj"] % 3]
                cnt["pj"] += 1
                MM([(PB[j][:, 0:t1 - t0], WIN[i][:, c, s_, :], HT[:, c, t0:t1], c == 0, c == 7) for c in range(8)],
                   [("WIN", i)] + tkeys("HT", t0, t1), [PBk[j]])
                return PB[j][:, 0:t1 - t0], PBk[j]

            def sigm(out, in_, R, W):
                A(out, in_, AF.Exp, R, W, scale=-1.0)
                A(out, out, AF.Ln, W + ["ONEC"], W, bias=ONEC[:, 0:1])
                A(out, out, AF.Exp, W, W, scale=-1.0)

            def chain(h, i, d, blk, first_writer):
                (t0, t1) = blk
                n = t1 - t0
                nch = n // 64
                ch0 = t0 // 64
                is_ctx = t0 >= NLAT
                a_i, z_i, m_i = (0, 63, 31) if d == 0 else (63, 0, 32)
                k = lambda nm: (nm, d)
                cv = lambda ap: ap[:, 0:n].rearrange("p (c j) -> p c j", j=64)
                bc = lambda v: v.unsqueeze(2).to_broadcast([128, nch, 64])
                pz, pk = proj(i, 2 + d, t0, t1)
                yield
                sigm(SG[d][:, 0:n], pz, [pk], [k("SG")])
                yield
                TS("dve", KK[d][:, 0:n], SG[d][:, 0:n], -1.0, LBM1[:, h, d:d + 1], ADD, MUL, [k("SG"), "LBM1"], [k("KK")])
                A(SG[d][:, 0:n], SG[d][:, 0:n], AF.Ln, [k("SG"), "OML", "LBT"], [k("SG")], scale=OML[:, h, d:d + 1], bias=LBT[:, h, d:d + 1])
                yield
                p.op("dve", (lambda: lambda e: e.tensor_tensor_scan(out=CIN[d][:, 0:n], data0=ONES32[:, 0:n], data1=SG[d][:, 0:n],
                                                                     initial=0.0, op0=MUL, op1=ADD))(), ["ONES32", k("SG")], [k("CIN")])
                yield
                if d == 1:
                    CP("dve", TOT[d][:], CIN[d][:, n - 1:n], [k("CIN")], [k("TOT")])
                    STT("dve", D1[d][:, 0:n], CIN[d][:, 0:n], -1.0, SG[d][:, 0:n], MUL, ADD, [k("CIN"), k("SG")], [k("D1")])
                    TS("dve", CIN[d][:, 0:n], D1[d][:, 0:n], TOT[d][:, 0:1], None, ADD, None, [k("D1"), k("TOT")], [k("CIN")])
                    yield
                TT("dve", PIN[d][:, 0:nch], cv(CIN[d])[:, :, a_i], cv(SG[d])[:, :, a_i], SUB, [k("CIN"), k("SG")], [k("PIN")])
                TT("dve", DCH[d][:, 0:nch], cv(CIN[d])[:, :, z_i], PIN[d][:, 0:nch], SUB, [k("CIN"), k("PIN")], [k("DCH")])
                A(DCH[d][:, 0:nch], DCH[d][:, 0:nch], AF.Exp, [k("DCH")], [k("DCH")])
                yield
                TT("dve", cv(D1[d]), cv(CIN[d]), bc(cv(CIN[d])[:, :, z_i]), SUB, [k("CIN")], [k("D1")])
                yield
                A(D1[d][:, 0:n], D1[d][:, 0:n], AF.Exp, [k("D1")], [k("D1")], scale=-1.0)
                yield
                TT("dve", KBT[d][:, 0:n], KK[d][:, 0:n], D1[d][:, 0:n], MUL, [k("KK"), k("D1")], [k("KBT")])
                yield
                TR([(PBH[0:64, kk_ * 128:(kk_ + 1) * 128], KBT[d][:, kk_ * 64:(kk_ + 1) * 64]) for kk_ in range(nch)], IDENT[:],
                   [k("KBT"), "IDENT"], [PBHk])
                yield
                CP("act", KBCH[d][0:64, 0:nch, :], PBH[0:64, 0:nch * 128].rearrange("p (c e) -> p c e", c=nch), [PBHk], [k("KBCH")])
                yield
                MM([(PB[2 + d][:, kk_ * 128:(kk_ + 1) * 128], KBCH[d][0:64, kk_, :], VCH[0:64, ch0 + kk_, :], True, True)
                    for kk_ in range(nch)], [k("KBCH")] + tkeys("VCH", t0, t1), [PBk[2 + d]])
                yield
                if not is_ctx:
                    TT("dve", cv(D1[d]), cv(CIN[d]), bc(PIN[d][:, 0:nch]), SUB, [k("CIN"), k("PIN")], [k("D1")])
                    yield
                    A(D1[d][:, 0:n], D1[d][:, 0:n], AF.Exp, [k("D1")], [k("D1")])
                    yield
                    TT("dve", QB[d][:, 0:n], QS[:, t0:t1], D1[d][:, 0:n], MUL, tkeys("QS", t0, t1) + [k("D1")], [k("QB")])
                    TT("dve", cv(D2[d]), cv(CIN[d]), bc(cv(CIN[d])[:, :, m_i]), SUB, [k("CIN")], [k("D2")])
                    yield
                    A(SG[d][:, 0:n], D2[d][:, 0:n], AF.Exp, [k("D2")], [k("SG")], scale=-1.0)
                    A(D2[d][:, 0:n], D2[d][:, 0:n], AF.Exp, [k("D2")], [k("D2")])
                    yield
                    TT("dve", QTL[d][:, 0:n], QS[:, t0:t1], D2[d][:, 0:n], MUL, tkeys("QS", t0, t1) + [k("D2")], [k("QTL")])
                    TT("dve", KTL[d][:, 0:n], KK[d][:, 0:n], SG[d][:, 0:n], MUL, [k("KK"), k("SG")], [k("KTL")])
                    yield
                    mms = []
                    for kk_ in range(nch):
                        c0 = kk_ * 64
                        if d == 0:
                            mms.append((PB[4][0:32, c0:c0 + 64], KTL[d][:, c0:c0 + 32], QTL[d][:, c0:c0 + 64], True, True))
                            mms.append((PB[4][32:64, c0 + 32:c0 + 64], KTL[d][:, c0 + 32:c0 + 64], QTL[d][:, c0 + 32:c0 + 64], True, True))
                        else:
                            mms.append((PB[5][0:32, c0:c0 + 32], KTL[d][:, c0:c0 + 32], QTL[d][:, c0:c0 + 32], True, True))
                            mms.append((PB[5][32:64, c0:c0 + 64], KTL[d][:, c0 + 32:c0 + 64], QTL[d][:, c0:c0 + 64], True, True))
                    MM(mms, [k("KTL"), k("QTL")], [PBk[4 + d]])
                    yield
                    TT("dve", AT[d][0:64, 0:n].rearrange("p (c j) -> p c j", j=64), PB[4 + d][0:64, 0:n].rearrange("p (c j) -> p c j", j=64),
                       MASK[d][:].unsqueeze(1).to_broadcast([64, nch, 64]), MUL, [PBk[4 + d], "MASK"], [k("AT")])
                    yield
                for kk_ in (range(nch) if d == 0 else range(nch - 1, -1, -1)):
                    if not is_ctx:
                        CP("act", SBF[d][:, kk_, :], S[d][:], [k("S")], [("SBF", d, kk_)])
                    STT("dve", S[d][:], S[d][:], DCH[d][:, kk_:kk_ + 1], PB[2 + d][:, kk_ * 128:(kk_ + 1) * 128], MUL, ADD,
                        [k("S"), k("DCH"), PBk[2 + d]], [k("S")])
                    yield
                if is_ctx:
                    return
                po = PB[4 + d][:, BL:BL + n]
                pok = PBk[4 + d]
                mms = []
                for kk_ in range(nch):
                    c0 = kk_ * 64
                    mms.append((PB[4 + d][:, BL + c0:BL + c0 + 64], SBF[d][:, kk_, :], QB[d][:, c0:c0 + 64], True, False))
                    mms.append((PB[4 + d][:, BL + c0:BL + c0 + 64], VCH[0:64, ch0 + kk_, :], AT[d][0:64, c0:c0 + 64], False, True))
                MM(mms, [("SBF", d, kk_) for kk_ in range(nch)] + [k("QB"), k("AT")] + tkeys("VCH", t0, t1), [pok])
                yield
                ok = [("OT", t0 // BL)]
                if first_writer:
                    CP("act", OT[:, t0:t1], po, [pok], ok)
                    yield
                    return
                TT("dve", OT[:, t0:t1], OT[:, t0:t1], po, ADD, [pok] + ok, ok)
                yield
                pg, pk = proj(i, 4, t0, t1)
                yield
                sigm(SGG[d][:, 0:n], pg, [pk], [k("SGG")])
                TT("dve", SGG[d][:, 0:n], SGG[d][:, 0:n], pg, MUL, [k("SGG"), pk], [k("SGG")])
                yield
                A(SQ[d][:, 0:n], OT[:, t0:t1], AF.Square, ok, [k("HSQ")])
                yield
                j_ = PJB[cnt["pj"] % 3]
                cnt["pj"] += 1
                pss, pssk = PB[j_][:, 0:n], PBk[j_]
                MM([(pss, ONESB[:], SQ[d][:, 0:n], True, True)], ["ONESB", k("HSQ")], [pssk])
                yield
                A(RS[d][:, 0:n], pss, AF.Ln, [pssk, "EPSC"], [k("HRS")], scale=1.0 / 128, bias=EPSC[:, 0:1])
                A(RS[d][:, 0:n], RS[d][:, 0:n], AF.Exp, [k("HRS")], [k("HRS")], scale=-0.5)
                yield
                STT("dve", D2[d][:, 0:n], OT[:, t0:t1], GOUT[:, 0:1], RS[d][:, 0:n], MUL, MUL, ok + ["GOUT", k("HRS")], [k("D2")])
                yield
                TT("dve", ONT[i][:, t0:t1], D2[d][:, 0:n], SGG[d][:, 0:n], MUL, [k("D2"), k("SGG")], tkeys(("HONT", i), t0, t1))
                yield

            def run_interleaved(gens):
                gens = list(gens)
                while gens:
                    for g in list(gens):
                        try:
                            next(g)
                        except StopIteration:
                            gens.remove(g)

            def gen_wo(hh):
                ii = hh % 2
                for t in range(16):
                    for dbl in range(2):
                        b_ = PJB[cnt["pj"] % 3]
                        cnt["pj"] += 1
                        ds = slice(dbl * 512, (dbl + 1) * 512)
                        MM([(PB[b_][:], ONT[ii][:, t * 128:(t + 1) * 128], WOG[ii][:, ds], True, True)], [(("HONT", ii), t), ("HWOG", ii)], [PBk[b_]])
                        TT("dve", X[:, t, ds], X[:, t, ds], PB[b_][:], ADD, [("X", t), PBk[b_]], [("X", t)])
                        yield

            for h in range(8):
                i = h % 2
                if h + 1 < 8:
                    load(h + 1)
                TT("pool", WOG[i][:], WO[i][:], G_l[:], MUL, [("HWO", i), "G_l"], [("HWOG", i)])
                blks = [ctx_block] + lat_blocks

                def v_tail(idx):
                    (t0, t1) = blks[idx]
                    n = t1 - t0
                    nch = n // 64
                    ch0 = t0 // 64
                    vb = idx % 2
                    TR([(PBH[0:64, kk_ * 128:(kk_ + 1) * 128], VTF[vb][:, kk_ * 64:(kk_ + 1) * 64]) for kk_ in range(nch)], IDENT[:],
                       [("VTF", vb), "IDENT"], [PBHk])
                    CP("dve", VCH[0:64, ch0:ch0 + nch, :], PBH[0:64, 0:nch * 128].rearrange("p (c e) -> p c e", c=nch), [PBHk],
                       tkeys("VCH", t0, t1))
                for idx, (t0, t1) in enumerate(blks):
                    n = t1 - t0
                    pq, pk = proj(i, 0, t0, t1)
                    A(QS[:, t0:t1], pq, AF.Silu, [pk], tkeys("QS", t0, t1))
                    pv, pk = proj(i, 1, t0, t1)
                    CP("dve", VTF[idx % 2][:, 0:n], pv, [pk], [("VTF", idx % 2)])
                    if idx >= 1:
                        v_tail(idx - 1)
                v_tail(len(blks) - 1)
                for d in range(2):
                    MEMSET("dve", S[d][:], 0.0, [("S", d)])
                bg = [gen_wo(h - 1)] if h > 0 else []
                for s_ in range(NB + 1):
                    if s_ == 0:
                        gens = [chain(h, i, 0, ctx_block, False), chain(h, i, 1, ctx_block, False)]
                    else:
                        fw = s_ <= NB // 2
                        gens = [chain(h, i, 0, lat_blocks[s_ - 1], fw), chain(h, i, 1, lat_blocks[NB - s_], fw)]
                    run_interleaved(gens + bg)
            run_interleaved([gen_wo(7)])
            p.barrier()

    out_streamed = [False]

    def finish():
        yv = yout.rearrange("(n p) d -> p n d", p=128)
        if not out_streamed[0]:
            for g in range(0, 16, 4):
                DMA("sp", "yout", [(yv[:, g:g + 4, :], X[:, g:g + 4, :])], [("X", t) for t in range(g, g + 4)], [])
        slots = ["yout"] + (["tap"] if B.tap_outs else [])
        p.emit(final_wait_slots=slots)
        return B

    norm_phase(0, "mix", NT)
    B.tap("HT0", HT[:], [128, 8, T], [("HT", t) for t in range(NT)], BF16)
    B.tap("G_l0", G_l[:], [128, D], ["G_l"])
    if stop == "norm0":
        return finish()
    attention_phase()
    B.tap("XMIX0", X[:], [128, NT, D], [("X", t) for t in range(NT)])
    if stop == "attn0":
        return finish()
    norm_phase(0, "ffn", NT)
    moe_phase(0, NT)
    B.tap("XFFN0", X[:], [128, NT, D], [("X", t) for t in range(NT)])
    if stop == "moe0":
        return finish()
    norm_phase(1, "mix", NT)
    hgrn_phase()
    B.tap("XMIX1", X[:], [128, NT, D], [("X", t) for t in range(NT)])
    if stop == "hgrn1":
        return finish()
    norm_phase(1, "ffn", 16)
    moe_phase(1, 16)
    return finish()


def _consts():
    ident = np.eye(128, dtype=np.float32)
    blk = np.zeros((128, 128), np.float32)
    blk[:64, :64] = 1.0
    blk[64:, 64:] = 1.0
    rm = np.zeros((128, 128), np.float32)
    for fp in range(128):
        j = fp % 64
        if j < 32:
            rm[fp + 32, fp] = -1.0
        else:
            rm[fp - 32, fp] = 1.0
    t = np.arange(NLAT)
    r = (t // 64).astype(np.float32)
    col = (t % 64).astype(np.float32)
    inv = (10000.0 ** (-np.arange(16, dtype=np.float32) / 16)).astype(np.float32)
    ang = np.concatenate([r[:, None] * inv, col[:, None] * inv], axis=-1).astype(np.float32)
    cosT = np.cos(ang).astype(np.float32).T
    sinT = np.sin(ang).astype(np.float32).T
    cos = np.tile(cosT, (4, 1))
    sin = np.tile(sinT, (4, 1))
    s_idx = np.arange(64)[:, None]
    t_idx = np.arange(64)[None, :]
    maskf = (s_idx <= t_idx).astype(np.float32)
    maskb = (s_idx >= t_idx).astype(np.float32)
    return dict(cst_ident=ident, cst_blk=blk, cst_rm=rm, cst_cos=np.ascontiguousarray(cos), cst_sin=np.ascontiguousarray(sin),
                cst_maskf=maskf, cst_maskb=maskb)


def make_in_maps(inputs):
    f = lambda a: np.ascontiguousarray(np.asarray(a, dtype=np.float32))
    shared = dict(
        ada_w=f(inputs["ada_w"]), ada_b=f(inputs["ada_b"]), norm_mix=f(inputs["norm_mix"]), norm_ffn=f(inputs["norm_ffn"]),
        attn_w_qkv=f(inputs["attn_w_qkv"])[0], attn_w_o=f(inputs["attn_w_o"])[0], attn_q_norm=f(inputs["attn_q_norm"])[0],
        attn_k_norm=f(inputs["attn_k_norm"])[0], attn_sub_norm=f(inputs["attn_sub_norm"])[0],
        attn_lambda=f(inputs["attn_lambda"])[0].reshape(256), hgrn_w_in=f(inputs["hgrn_w_in"])[0],
        hgrn_w_o=f(inputs["hgrn_w_o"])[0], hgrn_out_norm=f(inputs["hgrn_out_norm"])[0],
        hgrn_lb_gamma=f(inputs["hgrn_lb_gamma"]), router_w=f(inputs["router_w"]), router_bias=f(inputs["router_bias"]),
        moe_w_gate=f(inputs["moe_w_gate"]), moe_w_up=f(inputs["moe_w_up"]), moe_w_down=f(inputs["moe_w_down"]),
    )
    shared.update(_consts())
    x = f(inputs["x"])
    ctx = f(inputs["ctx"])
    c = f(inputs["c"])
    c_ctx = f(inputs["c_ctx"])
    maps = []
    for b in range(8):
        m = dict(shared)
        m["xin"] = np.ascontiguousarray(np.concatenate([x[b], ctx[b]], axis=0))
        m["cc"] = np.ascontiguousarray(np.stack([c[b], c_ctx], axis=0))
        maps.append(m)
    return maps


def kernel(**inputs):
    B = build()
    maps = make_in_maps(inputs)
    res = run_bass_kernel_spmd(B.nc, maps, core_ids=list(range(8)))
    return np.stack([np.asarray(r["y"], dtype=np.float32) for r in res.results], axis=0)
```

```python
import contextlib
import math

import numpy as np
import concourse.bass as bass
import concourse.mybir as mybir
from concourse.bass_utils import run_bass_kernel_spmd

F32 = mybir.dt.float32
BF16 = mybir.dt.bfloat16
AF = mybir.ActivationFunctionType
ALU = mybir.AluOpType
AX = mybir.AxisListType

D = 1024
NLAT = 2048
NCTX = 256
T = NLAT + NCTX
NT = T // 128
NE = 16
DFF = 512
EPS = 1e-6
BIG = 1.0e4


class _Op:
    __slots__ = ("eng", "fn", "deps", "is_dma", "slot", "ndma", "cum", "sig", "signals", "barriered")


class Prog:
    ENG = ("pe", "act", "dve", "pool", "sp")

    def __init__(self):
        self.nc = bass.Bass("TRN2", target_bir_lowering=False)
        self.ops = []
        self.last_w = {}
        self.readers = {}
        self.slot_cnt = {}
        self.last_on = {e: None for e in self.ENG}

    def _eng(self, e):
        nc = self.nc
        return {"pe": nc.tensor, "act": nc.scalar, "dve": nc.vector, "pool": nc.gpsimd, "sp": nc.sync}[e]

    def _deps(self, reads, writes):
        deps = set()
        for k in reads:
            w = self.last_w.get(k)
            if w is not None:
                deps.add(w)
        for k in writes:
            w = self.last_w.get(k)
            if w is not None:
                deps.add(w)
            for r in self.readers.get(k, ()):
                deps.add(r)
        return deps

    def _commit(self, oid, reads, writes):
        for k in reads:
            self.readers.setdefault(k, []).append(oid)
        for k in writes:
            self.last_w[k] = oid
            self.readers[k] = []

    def op(self, eng, fn, reads=(), writes=()):
        reads = list(reads)
        writes = list(writes)
        o = _Op()
        o.eng = eng
        o.fn = fn
        o.is_dma = False
        o.deps = self._deps(reads, writes)
        if eng == "pe":
            o.deps = {d for d in o.deps if self.ops[d].is_dma or self.ops[d].eng != "pe"}
        o.signals = False
        oid = len(self.ops)
        self.ops.append(o)
        self._commit(oid, reads, writes)
        self.last_on[eng] = oid
        return oid

    def dma(self, queue, slot, fn, reads=(), writes=(), ndma=1):
        reads = list(reads)
        writes = list(writes)
        o = _Op()
        o.eng = queue
        o.fn = fn
        o.is_dma = True
        o.slot = slot
        o.ndma = ndma
        o.barriered = False
        self.slot_cnt[slot] = self.slot_cnt.get(slot, 0) + ndma
        o.cum = self.slot_cnt[slot]
        o.deps = self._deps(reads, writes)
        o.signals = False
        oid = len(self.ops)
        self.ops.append(o)
        self._commit(oid, reads, writes)
        return oid

    def barrier(self):
        pend = set()
        for e in self.ENG:
            if self.last_on[e] is not None:
                pend.add(self.last_on[e])
        for i, o in enumerate(self.ops):
            if o.is_dma and not o.barriered:
                pend.add(i)
                o.barriered = True
        for e in self.ENG:
            o = _Op()
            o.eng = e
            o.fn = None
            o.is_dma = False
            o.deps = set(pend)
            o.signals = False
            self.ops.append(o)
        self.last_w = {}
        self.readers = {}

    def emit(self, final_wait_slots=()):
        nc = self.nc
        ops = self.ops
        for o in ops:
            for d in o.deps:
                if not ops[d].is_dma:
                    ops[d].signals = True
        self._stack = contextlib.ExitStack()
        esem = {e: self._stack.enter_context(nc.semaphore("s_" + e)) for e in self.ENG}
        ssem = {s: self._stack.enter_context(nc.semaphore("d_" + str(s))) for s in self.slot_cnt}
        cnt = {e: 0 for e in self.ENG}
        for o in ops:
            if not o.is_dma and o.signals:
                cnt[o.eng] += 1
                o.sig = cnt[o.eng]
        waited = {e: {} for e in self.ENG}
        for o in ops:
            eng = self._eng(o.eng)
            w = waited[o.eng]
            need = {}
            for d in o.deps:
                od = ops[d]
                if od.is_dma:
                    key = ("d", od.slot)
                    val = 16 * od.cum
                else:
                    key = ("e", od.eng)
                    val = od.sig
                if need.get(key, 0) < val:
                    need[key] = val
            for key, val in need.items():
                if w.get(key, 0) >= val:
                    continue
                sem = ssem[key[1]] if key[0] == "d" else esem[key[1]]
                eng.wait_ge(sem, val)
                w[key] = val
            if o.fn is None:
                continue
            if o.is_dma:
                insts = o.fn(eng)
                assert len(insts) == o.ndma, (len(insts), o.ndma)
                for ins in insts:
                    ins.then_inc(ssem[o.slot], 16)
            else:
                ins = o.fn(eng)
                if o.signals:
                    ins.then_inc(esem[o.eng], 1)
        for s in final_wait_slots:
            nc.sync.wait_ge(ssem[s], 16 * self.slot_cnt[s])
        return nc


class Builder:
    def __init__(self, stop=None, taps=()):
        self.p = Prog()
        self.nc = self.p.nc
        self.stop = stop
        self.taps = set(taps)
        self.tap_outs = []
        self.uid = 0

    def din(self, name, shape, dt=F32):
        return self.nc.dram_tensor(name, list(shape), dt, kind="ExternalInput").ap()

    def dout(self, name, shape, dt=F32):
        return self.nc.dram_tensor(name, list(shape), dt, kind="ExternalOutput").ap()

    def sb(self, st, name, shape, dt=F32):
        self.uid += 1
        return st.enter_context(self.nc.sbuf_tensor(f"{name}_{self.uid}", list(shape), dt))

    def tap(self, name, src_ap, shape, keys, dt=F32):
        if name not in self.taps:
            return
        o = self.dout("tap_" + name, shape, dt)
        self.p.dma("sp", "tap", lambda e: [e.dma_start(out=o, in_=src_ap)], reads=keys)
        self.tap_outs.append("tap_" + name)

    def A(self, out, in_, func, R, W, scale=None, bias=None, accum=None):
        kw = {}
        if scale is not None:
            kw["scale"] = scale
        if bias is not None:
            kw["bias"] = bias
        if accum is not None:
            kw["accum_out"] = accum
        self.p.op("act", lambda e: e.activation(out=out, in_=in_, func=func, **kw), R, W)

    def CP(self, eng, out, in_, R, W):
        if eng == "act":
            self.p.op("act", lambda e: e.copy(out=out, in_=in_), R, W)
        else:
            self.p.op(eng, lambda e: e.tensor_copy(out=out, in_=in_), R, W)

    def TT(self, eng, out, in0, in1, op, R, W):
        self.p.op(eng, lambda e: e.tensor_tensor(out=out, in0=in0, in1=in1, op=op), R, W)

    def TS(self, eng, out, in0, s1, s2, op0, op1, R, W):
        if s2 is None:
            self.p.op(eng, lambda e: e.tensor_scalar(out=out, in0=in0, scalar1=s1, scalar2=None, op0=op0), R, W)
        else:
            self.p.op(eng, lambda e: e.tensor_scalar(out=out, in0=in0, scalar1=s1, scalar2=s2, op0=op0, op1=op1), R, W)

    def STT(self, eng, out, in0, scalar, in1, op0, op1, R, W):
        self.p.op(eng, lambda e: e.scalar_tensor_tensor(out=out, in0=in0, scalar=scalar, in1=in1, op0=op0, op1=op1), R, W)

    def RED(self, out, in_, op, R, W):
        self.p.op("dve", lambda e: e.tensor_reduce(out=out, in_=in_, axis=AX.X, op=op), R, W)

    def RECIP(self, out, in_, R, W):
        self.p.op("dve", lambda e: e.reciprocal(out=out, in_=in_), R, W)

    def MEMSET(self, eng, ap, val, W):
        self.p.op(eng, lambda e: e.memset(ap, val), (), W)

    def MM(self, mms, R, W):
        mms = list(mms)

        def fn(e):
            ins = None
            for (o, l, r, s0, s1) in mms:
                ins = e.matmul(o, lhsT=l, rhs=r, start=s0, stop=s1)
            return ins
        self.p.op("pe", fn, R, W)

    def TR(self, trs, ident, R, W):
        trs = list(trs)

        def fn(e):
            ins = None
            for (o, i) in trs:
                ins = e.transpose(out=o, in_=i, identity=ident)
            return ins
        self.p.op("pe", fn, R, W)

    def DMA(self, queue, slot, pairs, R, W, slow=False):
        pairs = list(pairs)

        def fn(e):
            if slow:
                return [e.dma_start(out=o, in_=i, allow_slow_non_contiguous=True) for (o, i) in pairs]
            return [e.dma_start(out=o, in_=i) for (o, i) in pairs]
        self.p.dma(queue, slot, fn, R, W, ndma=len(pairs))


def tkeys(name, t0, t1):
    return [(name, t) for t in range(t0 // 128, (t1 + 127) // 128)]


def build(stop=None, taps=()):
    B = Builder(stop, taps)
    p, nc = B.p, B.nc
    A, CP, TT, TS, STT, RED, RECIP, MEMSET, MM, TR, DMA = B.A, B.CP, B.TT, B.TS, B.STT, B.RED, B.RECIP, B.MEMSET, B.MM, B.TR, B.DMA
    MUL, ADD, SUB, MAX, ISEQ = ALU.mult, ALU.add, ALU.subtract, ALU.max, ALU.is_equal

    xin = B.din("xin", [T, D])
    cc = B.din("cc", [2, D])
    ada_w = B.din("ada_w", [2, D, 6 * D])
    ada_b = B.din("ada_b", [2, 6 * D])
    norm_mix = B.din("norm_mix", [2, D])
    norm_ffn = B.din("norm_ffn", [2, D])
    w_qkv = B.din("attn_w_qkv", [D, 3 * D])
    w_ao = B.din("attn_w_o", [D, D])
    q_norm = B.din("attn_q_norm", [64])
    k_norm = B.din("attn_k_norm", [64])
    sub_norm = B.din("attn_sub_norm", [128])
    a_lambda = B.din("attn_lambda", [256])
    w_in = B.din("hgrn_w_in", [D, 5 * D])
    w_ho = B.din("hgrn_w_o", [D, D])
    out_norm = B.din("hgrn_out_norm", [128])
    lb_gamma = B.din("hgrn_lb_gamma", [2, 2, D])
    router_w = B.din("router_w", [D, NE])
    router_b = B.din("router_bias", [NE])
    w_gate = B.din("moe_w_gate", [2, NE, D, DFF])
    w_up = B.din("moe_w_up", [2, NE, D, DFF])
    w_down = B.din("moe_w_down", [2, NE, DFF, D])
    c_ident = B.din("cst_ident", [128, 128])
    c_blk = B.din("cst_blk", [128, 128])
    c_rm = B.din("cst_rm", [128, 128])
    c_cos = B.din("cst_cos", [128, NLAT])
    c_sin = B.din("cst_sin", [128, NLAT])
    c_maskf = B.din("cst_maskf", [64, 64])
    c_maskb = B.din("cst_maskb", [64, 64])
    yout = B.dout("y", [NLAT, D])

    top = contextlib.ExitStack()
    sb = B.sb
    X = sb(top, "X", [128, NT, D])
    HT = sb(top, "HT", [128, 8, T], BF16)
    G_l = sb(top, "G_l", [128, D])
    G_c = sb(top, "G_c", [128, D])
    IDENT = sb(top, "IDENT", [128, 128], BF16)
    ONESB = sb(top, "ONESB", [128, 128], BF16)
    BLK = sb(top, "BLK", [128, 128], BF16)
    RM = sb(top, "RM", [128, 128], BF16)
    EPSC = sb(top, "EPSC", [128, 1])
    CRL = sb(top, "CRL", [128, 8, 128], BF16)
    CRC = sb(top, "CRC", [128, 8, 128], BF16)
    GATES = sb(top, "GATES", [128, NT, NE])
    RB = sb(top, "RB", [128, NE])
    RWH = sb(top, "RWH", [128, 8, NE], BF16)
    RWL = sb(top, "RWL", [128, 8, NE], BF16)
    PB = [top.enter_context(nc.psum_tensor(f"PB{i}", [128, 512], F32)) for i in range(6)] + [None, None]
    PBHL = [None]
    PBk = [("PB", i) for i in range(8)]
    PBHk = ("PBH",)
    flex_ctr = [0]

    def flex_psum(st, all_f32):
        flex_ctr[0] += 1
        PB[6] = st.enter_context(nc.psum_tensor(f"PB6_{flex_ctr[0]}", [128, 512], F32))
        if all_f32:
            PB[7] = st.enter_context(nc.psum_tensor(f"PB7_{flex_ctr[0]}", [128, 512], F32))
            PBHL[0] = None
        else:
            PB[7] = None
            PBHL[0] = st.enter_context(nc.psum_tensor(f"PBH_{flex_ctr[0]}", [128, 1024], BF16))

    with contextlib.ExitStack() as st:
        C32 = sb(st, "C32", [128, 3, 128])
        CCT = sb(st, "CCT", [128, 8, 2])
        SCT = sb(st, "SCT", [128, 8, 2])
        RW32 = sb(st, "RW32", [128, 8, NE])
        RWT = sb(st, "RWT", [128, 8, NE])
        DMA("sp", "cst0", [(C32[:, 0, :], c_ident), (C32[:, 1, :], c_blk), (C32[:, 2, :], c_rm)], [], ["C32"])
        DMA("sp", "cst1", [(CCT[:, :, 0], cc[0].rearrange("(c p) -> p c", p=128)),
                          (CCT[:, :, 1], cc[1].rearrange("(c p) -> p c", p=128))], [], ["CCT"], slow=True)
        DMA("sp", "cst2", [(RW32[:], router_w.rearrange("(c p) e -> p c e", p=128)),
                          (RB[:], router_b.partition_broadcast(128))], [], ["RW32", "RB"])
        CP("dve", IDENT[:], C32[:, 0, :], ["C32"], ["IDENT"])
        CP("dve", BLK[:], C32[:, 1, :], ["C32"], ["BLK"])
        CP("dve", RM[:], C32[:, 2, :], ["C32"], ["RM"])
        MEMSET("dve", ONESB[:], 1.0, ["ONESB"])
        MEMSET("dve", EPSC[:], EPS, ["EPSC"])
        A(SCT[:], CCT[:], AF.Silu, ["CCT"], ["SCT"])
        CP("dve", CRL[:], SCT[:, :, 0:1].to_broadcast([128, 8, 128]), ["SCT"], ["CRL"])
        CP("dve", CRC[:], SCT[:, :, 1:2].to_broadcast([128, 8, 128]), ["SCT"], ["CRC"])
        CP("dve", RWH[:], RW32[:], ["RW32"], ["RWH"])
        TT("dve", RWT[:], RW32[:], RWH[:], SUB, ["RW32", "RWH"], ["RWT"])
        CP("dve", RWL[:], RWT[:], ["RWT"], ["RWL"])
        xv = xin.rearrange("(n p) d -> p n d", p=128)
        for g in range(0, NT, 3):
            DMA("sp", f"xld{g}", [(X[:, g:g + 3, :], xv[:, g:g + 3, :])], [], [("X", t) for t in range(g, g + 3)])
        p.barrier()

    def modslice(ADAW, BIAS, layer, j, out_l, out_c, defer=False):
        DMA("sp", "adab", [(BIAS[:], ada_b[layer, j * D:(j + 1) * D].partition_broadcast(128))], [], ["BIAS"])
        for half in range(2):
            cs_ = slice(j * D + half * 512, j * D + (half + 1) * 512)
            DMA("pool", f"adaw{half}", [(ADAW[half][:], ada_w[layer, :, cs_].rearrange("(c p) n -> p c n", p=128))], [], [("ADAW", half)])
        def compute():
            k = 0
            for half in range(2):
                for CR, crk, out in ((CRL, "CRL", out_l), (CRC, "CRC", out_c)):
                    if out is None:
                        continue
                    bank = k % 2
                    k += 1
                    MM([(PB[bank][:], CR[:, c, :], ADAW[half][:, c, :], c == 0, c == 7) for c in range(8)],
                       [crk, ("ADAW", half)], [PBk[bank]])
                    TT("dve", out[0][:, half * 512:(half + 1) * 512], PB[bank][:], BIAS[:, half * 512:(half + 1) * 512], ADD,
                       [PBk[bank], "BIAS"], [(out[1], half)])
        if defer:
            return compute
        compute()
        return None

    def norm_phase(layer, which, ntiles):
        j0 = 0 if which == "mix" else 3
        nw = norm_mix if which == "mix" else norm_ffn
        with_ctx = ntiles > 16
        with contextlib.ExitStack() as st:
            flex_psum(st, False)
            PBH = PBHL[0]
            ADAW = [sb(st, f"ADAW{i}", [128, 8, 512], BF16) for i in range(2)]
            BIAS = sb(st, "BIAS", [128, D])
            GN = sb(st, "GN", [128, D])
            SC_l = sb(st, "SC_l", [128, D])
            SC_c = sb(st, "SC_c", [128, D])
            A_l = sb(st, "A_l", [128, D])
            A_c = sb(st, "A_c", [128, D])
            SH_l = sb(st, "SH_l", [128, D])
            SH_c = sb(st, "SH_c", [128, D])
            TMP = [sb(st, f"TMP{i}", [128, D]) for i in range(2)]
            HB = [sb(st, f"HB{i}", [128, D], BF16) for i in range(2)]
            JUNK = HB[1]
            SS = sb(st, "SS", [128, NT])
            RSTD = sb(st, "RSTD", [128, NT])
            if which == "ffn":
                H32 = [sb(st, f"H32{i}", [128, D]) for i in range(2)]
                HLO = [sb(st, f"HLO{i}", [128, D], BF16) for i in range(2)]
                HTLO = [sb(st, f"HTLO{i}", [128, 8, 128], BF16) for i in range(2)]
                LG = sb(st, "LG", [128, NT, NE])
                RT = {n: sb(st, f"RT_{n}", [128, NT, NE]) for n in ("AFF", "BI", "EQ", "MB", "E1")}
                RS = {n: sb(st, f"RS_{n}", [128, NT, 4]) for n in ("M1", "M2", "GS", "PEN")}
                RV = {n: sb(st, f"RV_{n}", [128, NT]) for n in ("GM", "V1", "V2", "SUM")}
            DMA("sp", "gn", [(GN[:], nw[layer].partition_broadcast(128))], [], ["GN"])
            modslice(ADAW, BIAS, layer, j0 + 0, (SH_l, "SH_l"), (SH_c, "SH_c") if with_ctx else None)
            modslice(ADAW, BIAS, layer, j0 + 1, (SC_l, "SC_l"), (SC_c, "SC_c") if with_ctx else None)
            STT("dve", A_l[:], SC_l[:], 1.0, GN[:], ADD, MUL, [("SC_l", 0), ("SC_l", 1), "GN"], ["A_l"])
            if with_ctx:
                STT("dve", A_c[:], SC_c[:], 1.0, GN[:], ADD, MUL, [("SC_c", 0), ("SC_c", 1), "GN"], ["A_c"])
            gate_compute = modslice(ADAW, BIAS, layer, j0 + 2, (G_l, "G_l"), (G_c, "G_c") if with_ctx else None, defer=True)
            MEMSET("dve", SS[:], 0.0, ["SS"])
            for t in range(ntiles):
                A(JUNK[:], X[:, t, :], AF.Square, [("X", t), "SS"], [("HB", 1), "SS"], accum=SS[:, t:t + 1])
            A(RSTD[:, 0:ntiles], SS[:, 0:ntiles], AF.Sqrt, ["SS", "EPSC"], ["RSTD"], scale=1.0 / D, bias=EPSC[:, 0:1])
            RECIP(RSTD[:, 0:ntiles], RSTD[:, 0:ntiles], ["RSTD"], ["RSTD"])
            def stage_a(t):
                i = t % 2
                lat = t < 16
                Aw, Ak = (A_l, "A_l") if lat else (A_c, "A_c")
                Sw, Sk = (SH_l, "SH_l") if lat else (SH_c, "SH_c")
                Sks = [(Sk, 0), (Sk, 1)]
                STT("dve", TMP[i][:], X[:, t, :], RSTD[:, t:t + 1], Aw[:], MUL, MUL, [("X", t), "RSTD", Ak], [("TMP", i)])
                if which == "ffn" and t >= 1:
                    stage_b(t - 1)
                if which == "mix":
                    TT("pool" if t % 2 == 0 else "dve", HB[i][:], TMP[i][:], Sw[:], ADD, [("TMP", i)] + Sks, [("HB", i)])
                else:
                    TT("pool", H32[i][:], TMP[i][:], Sw[:], ADD, [("TMP", i)] + Sks, [("H32", i)])
                    CP("act", HB[i][:], H32[i][:], [("H32", i)], [("HB", i)])
                    TT("dve", HLO[i][:], H32[i][:], HB[i][:], SUB, [("H32", i), ("HB", i)], [("HLO", i)])
                TR([(PBH[:, c * 128:(c + 1) * 128], HB[i][:, c * 128:(c + 1) * 128]) for c in range(8)], IDENT[:],
                   [("HB", i), "IDENT"], [PBHk])
                CP("act", HT[:, :, t * 128:(t + 1) * 128], PBH[:].rearrange("p (c j) -> p c j", c=8), [PBHk], [("HT", t)])
                if which == "ffn":
                    TR([(PBH[:, c * 128:(c + 1) * 128], HLO[i][:, c * 128:(c + 1) * 128]) for c in range(8)], IDENT[:],
                       [("HLO", i), "IDENT"], [PBHk])
                    CP("act", HTLO[i][:], PBH[:].rearrange("p (c j) -> p c j", c=8), [PBHk], [("HTLO", i)])

            def stage_b(t):
                i = t % 2
                lg = PB[2 + i][:, 0:NE]
                mms = []
                for c in range(8):
                    mms.append((lg, HT[:, c, t * 128:(t + 1) * 128], RWH[:, c, :], c == 0, False))
                    mms.append((lg, HTLO[i][:, c, :], RWH[:, c, :], False, False))
                    mms.append((lg, HT[:, c, t * 128:(t + 1) * 128], RWL[:, c, :], False, c == 7))
                MM(mms, [("HT", t), ("HTLO", i), "RWH", "RWL"], [PBk[2 + i]])
                CP("act", LG[:, t, :], lg, [PBk[2 + i]], [("LG", t)])

            def router_all(nt):
                f3 = lambda ap: ap[:, 0:nt, :]
                v4 = lambda ap: ap[:, 0:nt, :].rearrange("p t (g j) -> p t g j", g=4)
                g3 = lambda ap: ap[:, 0:nt, :]
                v2 = lambda ap: ap[:, 0:nt]
                bj = lambda ap: ap[:, 0:nt, :].unsqueeze(3).to_broadcast([128, nt, 4, 4])
                be = lambda ap: ap[:, 0:nt].unsqueeze(2).to_broadcast([128, nt, NE])
                bg4 = lambda ap: ap[:, 0:nt].unsqueeze(2).to_broadcast([128, nt, 4])
                AFF, BI, EQ, MB, E1 = (RT[n] for n in ("AFF", "BI", "EQ", "MB", "E1"))
                E2 = EQ
                M1, M2, GS, PEN = (RS[n] for n in ("M1", "M2", "GS", "PEN"))
                GM, V1, V2, SUM = (RV[n] for n in ("GM", "V1", "V2", "SUM"))
                A(f3(AFF), f3(LG), AF.Sigmoid, [("LG", t) for t in range(nt)], ["AFF"])
                TT("dve", f3(BI), f3(AFF), RB[:].unsqueeze(1).to_broadcast([128, nt, NE]), ADD, ["AFF", "RB"], ["BI"])
                RED(g3(M1), v4(BI), MAX, ["BI"], ["M1"])
                TT("dve", v4(EQ), v4(BI), bj(M1), ISEQ, ["BI", "M1"], ["EQ"])
                STT("dve", f3(EQ), f3(EQ), -BIG, f3(BI), MUL, ADD, ["EQ", "BI"], ["EQ"])
                RED(g3(M2), v4(EQ), MAX, ["EQ"], ["M2"])
                TT("dve", g3(GS), g3(M1), g3(M2), ADD, ["M1", "M2"], ["GS"])
                RED(v2(GM), g3(GS), MAX, ["GS"], ["GM"])
                TT("dve", g3(M1), g3(GS), bg4(GM), ISEQ, ["GS", "GM"], ["M1"])
                TS("dve", g3(PEN), g3(M1), -1.0, BIG, ADD, MUL, ["M1"], ["PEN"])
                TT("dve", v4(MB), v4(BI), bj(PEN), ADD, ["BI", "PEN"], ["MB"])
                RED(v2(V1), f3(MB), MAX, ["MB"], ["V1"])
                TT("dve", f3(E1), f3(MB), be(V1), ISEQ, ["MB", "V1"], ["E1"])
                STT("dve", f3(MB), f3(E1), -BIG, f3(MB), MUL, ADD, ["E1", "MB"], ["MB"])
                RED(v2(V2), f3(MB), MAX, ["MB"], ["V2"])
                TT("dve", f3(E2), f3(MB), be(V2), ISEQ, ["MB", "V2"], ["EQ"])
                TT("dve", f3(E1), f3(E1), f3(E2), ADD, ["E1", "EQ"], ["E1"])
                TT("dve", f3(E1), f3(AFF), f3(E1), MUL, ["AFF", "E1"], ["E1"])
                RED(v2(SUM), f3(E1), ADD, ["E1"], ["SUM"])
                RECIP(v2(SUM), v2(SUM), ["SUM"], ["SUM"])
                TT("dve", GATES[:, 0:nt, :], f3(E1), be(SUM), MUL, ["E1", "SUM"], [("GATES", t) for t in range(nt)])

            for t in range(ntiles):
                stage_a(t)
            gate_compute()
            if which == "ffn":
                stage_b(ntiles - 1)
                router_all(ntiles)
            p.barrier()


    def attention_phase():
        blocks = [(0, 512), (512, 1024), (1024, 1536), (1536, 2048), (2048, 2304)]
        with contextlib.ExitStack() as st:
            flex_psum(st, True)
            CS = [sb(st, f"CS{i}", [128, 2, 512]) for i in range(1)]
            WQKV = sb(st, "WQKV", [128, 8, 3, 128], BF16)
            WO = [sb(st, f"WO{i}", [128, D], BF16) for i in range(2)]
            WOG = [[sb(st, f"WOG{i}{g}", [128, D], BF16) for g in range(2)] for i in range(2)]
            QT = [sb(st, f"QT{i}", [128, T], BF16) for i in range(2)]
            KT = [sb(st, f"KT{i}", [128, T], BF16) for i in range(2)]
            VT = [sb(st, f"VT{i}", [128, NT, 128], BF16) for i in range(2)]
            ONT = sb(st, "ONT", [128, T], BF16)
            GQ = sb(st, "GQ", [128, 1])
            GK = sb(st, "GK", [128, 1])
            GSUB = sb(st, "GSUB", [128, 1])
            LAMB = sb(st, "LAMB", [128, 256])
            LTMP = sb(st, "LTMP", [128, 128])
            LS = sb(st, "LS", [128, 2])
            NLAM = sb(st, "NLAM", [128, 1])
            SQ = sb(st, "SQ", [128, 512], BF16)
            RST = sb(st, "RST", [128, 512])
            QN = sb(st, "QN", [128, 512], BF16)
            T1 = sb(st, "T1", [128, 512])
            T2 = sb(st, "T2", [128, 512])
            E = [sb(st, f"E{i}", [128, 512], BF16) for i in range(4)]
            ZS = [[sb(st, f"ZS{b}{i}", [128, 512]) for i in range(2)] for b in range(2)]
            OS = [[sb(st, f"OS{b}{i}", [128, 512]) for i in range(2)] for b in range(2)]

            col = lambda v: v.rearrange("(p o) -> p o", o=1)
            DMA("sp", "acst0", [(GQ[0:64, :], col(q_norm)), (GQ[64:128, :], col(q_norm)), (GK[0:64, :], col(k_norm)),
                                (GK[64:128, :], col(k_norm)), (GSUB[:], col(sub_norm)),
                                (LAMB[:], a_lambda.partition_broadcast(128))], [], ["GQ", "GK", "GSUB", "LAMB"])
            lam_init0 = 0.8 - 0.6 * math.exp(0.0)
            TS("dve", GSUB[:], GSUB[:], 1.0 - lam_init0, None, MUL, None, ["GSUB"], ["GSUB"])
            lv = LAMB[:].rearrange("p (a b d) -> p a b d", a=2, b=2)
            TT("dve", LTMP[:].rearrange("p (a d) -> p a d", a=2), lv[:, :, 0, :], lv[:, :, 1, :], MUL, ["LAMB"], ["LTMP"])
            RED(LS[:], LTMP[:].rearrange("p (a d) -> p a d", a=2), ADD, ["LTMP"], ["LS"])
            A(LS[:], LS[:], AF.Exp, ["LS"], ["LS"])
            TT("dve", NLAM[:], LS[:, 1:2], LS[:, 0:1], SUB, ["LS"], ["NLAM"])
            TS("dve", NLAM[:], NLAM[:], -lam_init0, None, ADD, None, ["NLAM"], ["NLAM"])

            cnt = {"e": 0, "cs": 0}
            qdone = {}

            def gen_proj(h):
                par = h % 2

                def load_w(hh):
                    pp = hh % 2
                    DMA("pool", "wqkv", [(WQKV[:, :, s_, :], w_qkv[:, s_ * D + hh * 128:s_ * D + (hh + 1) * 128]
                                          .rearrange("(c p) f -> p c f", p=128)) for s_ in range(3)], [], ["WQKV"])
                    DMA("pool", f"wo{pp}", [(WO[pp][:], w_ao[hh * 128:(hh + 1) * 128, :])], [], [("WO", pp)])
                if h == 0:
                    load_w(0)
                yield

                def qk(s_, dst, gcol, gk, dkey, t0, t1):
                    n = t1 - t0
                    pq = PB[2][:, 0:n]
                    MM([(pq, WQKV[:, c, s_, :], HT[:, c, t0:t1], c == 0, c == 7) for c in range(8)],
                       ["WQKV"] + tkeys("HT", t0, t1), [PBk[2]])
                    yield
                    A(SQ[:, 0:n], pq, AF.Square, [PBk[2]], ["SQ"])
                    yield
                    yield
                    MM([(PB[3][:, 0:n], BLK[:], SQ[:, 0:n], True, True)], ["BLK", "SQ"], [PBk[3]])
                    A(RST[:, 0:n], PB[3][:, 0:n], AF.Ln, [PBk[3], "EPSC"], ["RST"], scale=1.0 / 64, bias=EPSC[:, 0:1])
                    A(RST[:, 0:n], RST[:, 0:n], AF.Exp, ["RST"], ["RST"], scale=-0.5)
                    yield
                    if t0 < NLAT:
                        cs = 0
                        DMA("sp", f"cs{cs}", [(CS[cs][:, 0, 0:n], c_cos[:, t0:t1]), (CS[cs][:, 1, 0:n], c_sin[:, t0:t1])], [], [("CS", cs)])
                        STT("dve", QN[:, 0:n], pq, gcol[:, 0:1], RST[:, 0:n], MUL, MUL, [PBk[2], gk, "RST"], ["QN"])
                        yield
                        yield
                        MM([(PB[3][:, 0:n], RM[:], QN[:, 0:n], True, True)], ["RM", "QN"], [PBk[3]])
                        TT("pool", T1[:, 0:n], QN[:, 0:n], CS[cs][:, 0, 0:n], MUL, ["QN", ("CS", cs)], ["T1"])
                        yield
                        TT("dve", T2[:, 0:n], PB[3][:, 0:n], CS[cs][:, 1, 0:n], MUL, [PBk[3], ("CS", cs)], ["T2"])
                        TT("pool", dst[:, t0:t1], T1[:, 0:n], T2[:, 0:n], ADD, ["T1", "T2"], tkeys(dkey, t0, t1))
                        yield True
                    else:
                        STT("dve", dst[:, t0:t1], pq, gcol[:, 0:1], RST[:, 0:n], MUL, MUL, [PBk[2], gk, "RST"], tkeys(dkey, t0, t1))
                        yield True
                for (t0, t1) in blocks:
                    yield from qk(1, KT[par], GK, "GK", ("KT", par), t0, t1)
                for g in range(0, NT, 4):
                    ng = min(4, NT - g)
                    mms = []
                    for k_ in range(ng):
                        tt = g + k_
                        mms += [(PB[2][:, k_ * 128:(k_ + 1) * 128], HT[:, c, tt * 128:(tt + 1) * 128], WQKV[:, c, 2, :], c == 0, c == 7)
                                for c in range(8)]
                    MM(mms, ["WQKV"] + [("HT", g + k_) for k_ in range(ng)], [PBk[2]])
                    yield
                    CP("dve", VT[par][:, g:g + ng, :], PB[2][:, 0:ng * 128].rearrange("p (a e) -> p a e", a=ng), [PBk[2]],
                       [(("VT", par), g + k_) for k_ in range(ng)])
                    yield True
                TT("pool", WOG[par][0][:], WO[par][:], G_l[:], MUL, [("WO", par), "G_l"], [("WOG", par, 0)])
                yield True
                TT("pool", WOG[par][1][:], WO[par][:], G_c[:], MUL, [("WO", par), "G_c"], [("WOG", par, 1)])
                yield True
                for bi, (t0, t1) in enumerate(blocks):
                    yield from qk(0, QT[par], GQ, "GQ", ("QT", par), t0, t1)
                if h + 1 < 8:
                    load_w(h + 1)
                    yield True

            def gen_wo(h):
                par = h % 2
                for t in range(NT):
                    gi = 0 if t < 16 else 1
                    for dbl in range(2):
                        ds = slice(dbl * 512, (dbl + 1) * 512)
                        b_ = 2 + dbl
                        MM([(PB[b_][:], ONT[:, t * 128:(t + 1) * 128], WOG[par][gi][:, ds], True, True)], [("ONT", t), ("WOG", par, gi)], [PBk[b_]])
                        TT("dve", X[:, t, ds], X[:, t, ds], PB[b_][:], ADD, [("X", t), PBk[b_]], [("X", t)])
                        yield True

            def gen_attn(h):
                par = h % 2
                for bi, (q0, q1) in enumerate(blocks):
                    nq = q1 - q0
                    ktiles = list(range(NT)) if q0 < NLAT else [16, 17]
                    pend = []

                    def av(ki, kt, ebs):
                        first, last = ki == 0, ki == len(ktiles) - 1
                        MM([(PB[4][:, 0:nq], VT[par][:, kt, :], E[ebs[0]][:, 0:nq], first, last),
                            (PB[5][:, 0:nq], VT[par][:, kt, :], E[ebs[1]][:, 0:nq], first, last),
                            (PB[6][:, 0:nq], ONESB[:], E[ebs[0]][:, 0:nq], first, last),
                            (PB[7][:, 0:nq], ONESB[:], E[ebs[1]][:, 0:nq], first, last)],
                           [(("VT", par), kt), ("E", ebs[0]), ("E", ebs[1]), "ONESB"], [PBk[4], PBk[5], PBk[6], PBk[7]])
                    for ki, kt in enumerate(ktiles):
                        ebs = (cnt["e"] % 4, (cnt["e"] + 1) % 4)
                        cnt["e"] += 2
                        ks = slice(kt * 128, (kt + 1) * 128)
                        for m in range(2):
                            MM([(PB[m][:, 0:nq], KT[par][m * 64:(m + 1) * 64, ks], QT[par][m * 64:(m + 1) * 64, q0:q1], True, True)],
                               [(("KT", par), kt)] + tkeys(("QT", par), q0, q1), [PBk[m]])
                            A(E[ebs[m]][:, 0:nq], PB[m][:, 0:nq], AF.Exp, [PBk[m]], [("E", ebs[m])], scale=0.125)
                        if pend:
                            av(*pend.pop())
                        pend.append((ki, kt, ebs))
                        yield
                    qdone[h] = bi + 1
                    av(*pend.pop())
                    yield
                    zb = bi % 2
                    for m in range(2):
                        CP("dve", ZS[zb][m][:, 0:nq], PB[6 + m][:, 0:nq], [PBk[6 + m]], [("ZS", zb, m)])
                        CP("act", OS[zb][m][:, 0:nq], PB[4 + m][:, 0:nq], [PBk[4 + m]], [("OS", zb, m)])
                    prio.append(gen_epi(zb, q0, q1))
                    yield

            def gen_epi(zb, q0, q1):
                nq = q1 - q0
                Z, O = ZS[zb], OS[zb]
                for m in range(2):
                    A(Z[m][:, 0:nq], Z[m][:, 0:nq], AF.Ln, [("ZS", zb, m)], [("ZS", zb, m)])
                    A(Z[m][:, 0:nq], Z[m][:, 0:nq], AF.Exp, [("ZS", zb, m)], [("ZS", zb, m)], scale=-1.0)
                    yield
                    TT("pool", O[m][:, 0:nq], O[m][:, 0:nq], Z[m][:, 0:nq], MUL, [("OS", zb, m), ("ZS", zb, m)], [("OS", zb, m)])
                    yield
                STT("dve", O[0][:, 0:nq], O[1][:, 0:nq], NLAM[:, 0:1], O[0][:, 0:nq], MUL, ADD, [("OS", zb, 0), ("OS", zb, 1), "NLAM"], [("OS", zb, 0)])
                yield
                A(SQ[:, 0:nq], O[0][:, 0:nq], AF.Square, [("OS", zb, 0)], ["SQ"])
                yield
                yield
                MM([(PB[3][:, 0:nq], ONESB[:], SQ[:, 0:nq], True, True)], ["ONESB", "SQ"], [PBk[3]])
                A(RST[:, 0:nq], PB[3][:, 0:nq], AF.Ln, [PBk[3], "EPSC"], ["RST"], scale=1.0 / 128, bias=EPSC[:, 0:1])
                A(RST[:, 0:nq], RST[:, 0:nq], AF.Exp, ["RST"], ["RST"], scale=-0.5)
                yield
                STT("dve", ONT[:, q0:q1], O[0][:, 0:nq], GSUB[:, 0:1], RST[:, 0:nq], MUL, MUL, [("OS", zb, 0), "GSUB", "RST"], tkeys("ONT", q0, q1))
                yield

            bgq = []
            prio = []
            BG_PER_MAIN = 2

            rs_state = {"cur": None, "cur_prio": False}

            def run_stage(main, drain_all=False):
                main_alive = main is not None
                cur, cur_prio = rs_state["cur"], rs_state["cur_prio"]
                while main_alive or bgq or (cur is not None and not cur_prio) or (drain_all and (prio or cur is not None)):
                    if main_alive:
                        try:
                            next(main)
                        except StopIteration:
                            main_alive = False
                    for _rep in range(BG_PER_MAIN):
                        if cur is None:
                            if prio:
                                cur, cur_prio = prio.pop(0), True
                            elif bgq:
                                cur, cur_prio = bgq[0], False
                        if cur is not None:
                            try:
                                r = next(cur)
                            except StopIteration:
                                if not cur_prio:
                                    bgq.pop(0)
                                cur = None
                                continue
                            if r is True and not cur_prio and prio:
                                cur = None
                rs_state["cur"], rs_state["cur_prio"] = cur, cur_prio

            bgq.append(gen_proj(0))
            run_stage(None)
            for h in range(8):
                if h > 0:
                    bgq.append(gen_wo(h - 1))
                if h + 1 < 8:
                    bgq.append(gen_proj(h + 1))
                run_stage(gen_attn(h))
            bgq.append(gen_wo(7))
            run_stage(None, drain_all=True)
            p.barrier()

    def moe_phase(layer, ntiles):
        blocks = [(0, 512), (512, 1024), (1024, 1536), (1536, 2048)] + ([(2048, 2304)] if ntiles > 16 else [])
        with contextlib.ExitStack() as st:
            flex_psum(st, False)
            WG = [sb(st, f"WG{i}", [128, 8, DFF], BF16) for i in range(2)]
            WU = [sb(st, f"WU{i}", [128, 8, DFF], BF16) for i in range(2)]
            WD = [sb(st, f"WD{i}", [128, 4, D], BF16) for i in range(2)]
            SG = [sb(st, f"SG{i}", [128, 512]) for i in range(2)]
            HID = [sb(st, f"HID{i}", [128, 4, 512], BF16) for i in range(2)]
            WT = [sb(st, f"WT{i}", [128, 512]) for i in range(2)]

            def load(e):
                i = e % 2
                gk = [("WG", i, f_) for f_ in range(4)]
                uk = [("WU", i, f_) for f_ in range(4)]
                if e == 0:
                    for f_ in range(4):
                        fs_ = slice(f_ * 128, (f_ + 1) * 128)
                        DMA("pool", f"wg0c{f_}", [(WG[i][:, :, fs_], w_gate[layer, e][:, fs_].rearrange("(c p) f -> p c f", p=128))], [], [gk[f_]])
                        DMA("pool", f"wu0c{f_}", [(WU[i][:, :, fs_], w_up[layer, e][:, fs_].rearrange("(c p) f -> p c f", p=128))], [], [uk[f_]])
                else:
                    DMA("pool", f"wg{i}", [(WG[i][:], w_gate[layer, e].rearrange("(c p) f -> p c f", p=128))], [], gk)
                    DMA("pool", f"wu{i}", [(WU[i][:], w_up[layer, e].rearrange("(c p) f -> p c f", p=128))], [], uk)
                DMA("pool", f"wd{i}", [(WD[i][:], w_down[layer, e].rearrange("(c p) n -> p c n", p=128))], [], [("WD", i)])
            load(0)
            cnt = {"c2": 0, "c3": 0}

            def gate_up(e, blk, hb):
                i = e % 2
                (t0, t1) = blk
                n = t1 - t0
                for ffc in range(4):
                    j = cnt["c2"] % 2
                    cnt["c2"] += 1
                    fs = slice(ffc * 128, (ffc + 1) * 128)
                    MM([(PB[j][:, 0:n], WG[i][:, c, fs], HT[:, c, t0:t1], c == 0, c == 7) for c in range(8)],
                       [("WG", i, ffc)] + tkeys("HT", t0, t1), [PBk[j]])
                    MM([(PB[2 + j][:, 0:n], WU[i][:, c, fs], HT[:, c, t0:t1], c == 0, c == 7) for c in range(8)],
                       [("WU", i, ffc)] + tkeys("HT", t0, t1), [PBk[2 + j]])
                    A(SG[j][:, 0:n], PB[j][:, 0:n], AF.Silu, [PBk[j]], [("SG", j)])
                    TT("dve", HID[hb][:, ffc, 0:n], SG[j][:, 0:n], PB[2 + j][:, 0:n], MUL, [("SG", j), PBk[2 + j]], [("HID", hb)])

            def down(e, blk, hb):
                i = e % 2
                (t0, t1) = blk
                for tt in range(t0 // 128, t1 // 128):
                    Gw, Gk_ = (G_l, "G_l") if tt < 16 else (G_c, "G_c")
                    o0 = tt * 128 - t0
                    for dbl in range(2):
                        b_ = 4 + (cnt["c3"] % 3)
                        w_ = cnt["c3"] % 2
                        cnt["c3"] += 1
                        ds = slice(dbl * 512, (dbl + 1) * 512)
                        MM([(PB[b_][:], HID[hb][:, ffc, o0:o0 + 128], WD[i][:, ffc, ds], ffc == 0, ffc == 3) for ffc in range(4)],
                           [("HID", hb), ("WD", i)], [PBk[b_]])
                        STT("dve", WT[w_][:], PB[b_][:], GATES[:, tt, e:e + 1], Gw[:, ds], MUL, MUL,
                            [PBk[b_], ("GATES", tt), Gk_], [("WT", w_)])
                        TT("pool", X[:, tt, ds], X[:, tt, ds], WT[w_][:], ADD, [("X", tt), ("WT", w_)], [("X", tt)])
                    if layer == 1 and e == NE - 1 and tt < 16:
                        DMA("sp", "yout", [(yout.rearrange("(n p) d -> p n d", p=128)[:, tt:tt + 1, :], X[:, tt:tt + 1, :])], [("X", tt)], [])
                        out_streamed[0] = True

            items = [(e, blk) for e in range(NE) for blk in blocks]
            for k_, (e, blk) in enumerate(items):
                gate_up(e, blk, k_ % 2)
                if k_ >= 1:
                    pe_, pblk = items[k_ - 1]
                    down(pe_, pblk, (k_ - 1) % 2)
                if blk == blocks[0] and e + 1 < NE:
                    load(e + 1)
            pe_, pblk = items[-1]
            down(pe_, pblk, (len(items) - 1) % 2)
            p.barrier()

    def hgrn_phase():
        BL = 256
        NCH = BL // 64
        lat_blocks = [(b * BL, (b + 1) * BL) for b in range(NLAT // BL)]
        ctx_block = (NLAT, T)
        NB = len(lat_blocks)
        with contextlib.ExitStack() as st:
            flex_psum(st, False)
            PBH = PBHL[0]
            WIN = [sb(st, f"WIN{i}", [128, 8, 5, 128], BF16) for i in range(2)]
            WO = [sb(st, f"HWO{i}", [128, D], BF16) for i in range(2)]
            WOG = [sb(st, f"HWOG{i}", [128, D], BF16) for i in range(2)]
            GAM = sb(st, "GAM", [128, 8, 2, 2])
            LBT = sb(st, "LBT", [128, 8, 2])
            OML = sb(st, "OML", [128, 8, 2])
            LBM1 = sb(st, "LBM1", [128, 8, 2])
            GOUT = sb(st, "GOUT", [128, 1])
            ONEC = sb(st, "ONEC", [128, 1])
            MASK = [sb(st, f"MASK{i}", [64, 64]) for i in range(2)]
            ONES32 = sb(st, "ONES32", [128, BL])
            QS = sb(st, "QS", [128, T])
            VCH = sb(st, "VCH", [64, T // 64, 128], BF16)
            OT = X[:, 16:18, :].rearrange("p a d -> p (a d)")
            ONT = [sb(st, f"HONT{i}", [128, NLAT], BF16) for i in range(2)]
            VTF = [sb(st, f"VTF{i}", [128, BL], BF16) for i in range(2)]
            S = [sb(st, f"S{i}", [128, 128]) for i in range(2)]
            tmp = lambda nm, shape, dt=F32: [sb(st, f"{nm}{i}", shape, dt) for i in range(2)]
            SBF = tmp("SBF", [128, NCH, 128], BF16)
            SG = tmp("HSG", [128, BL])
            KK = tmp("KK", [128, BL])
            CIN = tmp("CIN", [128, BL])
            D1 = tmp("D1", [128, BL])
            D2 = tmp("D2", [128, BL])
            QB = tmp("QB", [128, BL], BF16)
            QTL = tmp("QTL", [128, BL], BF16)
            KTL = tmp("KTL", [128, BL], BF16)
            KBT = tmp("KBT", [128, BL], BF16)
            KBCH = tmp("KBCH", [64, NCH, 128], BF16)
            AT = tmp("AT", [64, BL], BF16)
            PIN = tmp("PIN", [128, NCH])
            DCH = tmp("DCH", [128, NCH])
            TOT = tmp("TOT", [128, 1])
            SGG = tmp("SGG", [128, BL])
            SQ = tmp("HSQ", [128, BL], BF16)
            RS = tmp("HRS", [128, BL])

            for a_ in range(2):
                for l_ in range(2):
                    DMA("sp", f"hcst{a_}{l_}", [(GAM[:, :, a_, l_], lb_gamma[a_, l_].rearrange("(h p) -> p h", p=128))], [], ["GAM"], slow=True)
            DMA("sp", "hcstm", [(GOUT[:], out_norm.rearrange("(p o) -> p o", o=1)), (MASK[0][:], c_maskf), (MASK[1][:], c_maskb)],
                [], ["GOUT", "MASK"])
            TT("dve", LBT[:], GAM[:, :, :, 1], GAM[:, :, :, 0], SUB, ["GAM"], ["LBT"])
            MEMSET("dve", ONEC[:], 1.0, ["ONEC"])
            A(LBT[:], LBT[:], AF.Exp, ["LBT"], ["LBT"], scale=-1.0)
            A(LBT[:], LBT[:], AF.Ln, ["LBT", "ONEC"], ["LBT"], bias=ONEC[:, 0:1])
            A(LBT[:], LBT[:], AF.Exp, ["LBT"], ["LBT"], scale=-1.0)
            TS("dve", OML[:], LBT[:], -1.0, 1.0, MUL, ADD, ["LBT"], ["OML"])
            TS("dve", LBM1[:], LBT[:], -1.0, None, ADD, None, ["LBT"], ["LBM1"])
            MEMSET("dve", ONES32[:], 1.0, ["ONES32"])
            MEMSET("dve", PB[4][:], 0.0, [PBk[4]])
            MEMSET("dve", PB[5][:], 0.0, [PBk[5]])

            def load(h):
                i = h % 2
                DMA("pool", f"win{i}", [(WIN[i][:, :, s_, :], w_in[:, s_ * D + h * 128:s_ * D + (h + 1) * 128]
                                         .rearrange("(c p) f -> p c f", p=128)) for s_ in range(5)], [], [("WIN", i)])
                DMA("pool", f"hwo{i}", [(WO[i][:], w_ho[h * 128:(h + 1) * 128, :])], [], [("HWO", i)])
            load(0)
            cnt = {"pj": 0, "w": 0}

            PJB = [0, 1, 6]

            def proj(i, s_, t0, t1):
                j = PJB[cnt["pj"] % 3]
                cnt["pj"] += 1
                MM([(PB[j][:, 0:t1 - t0], WIN[i][:, c, s_, :], HT[:, c, t0:t1], c == 0, c == 7) for c in range(8)],
                   [("WIN", i)] + tkeys("HT", t0, t1), [PBk[j]])
                return PB[j][:, 0:t1 - t0], PBk[j]

            def sigm(out, in_, R, W):
                A(out, in_, AF.Exp, R, W, scale=-1.0)
                A(out, out, AF.Ln, W + ["ONEC"], W, bias=ONEC[:, 0:1])
                A(out, out, AF.Exp, W, W, scale=-1.0)

            def chain(h, i, d, blk, first_writer):
                (t0, t1) = blk
                n = t1 - t0
                nch = n // 64
                ch0 = t0 // 64
                is_ctx = t0 >= NLAT
                a_i, z_i, m_i = (0, 63, 31) if d == 0 else (63, 0, 32)
                k = lambda nm: (nm, d)
                cv = lambda ap: ap[:, 0:n].rearrange("p (c j) -> p c j", j=64)
                bc = lambda v: v.unsqueeze(2).to_broadcast([128, nch, 64])
                pz, pk = proj(i, 2 + d, t0, t1)
                yield
                sigm(SG[d][:, 0:n], pz, [pk], [k("SG")])
                yield
                TS("dve", KK[d][:, 0:n], SG[d][:, 0:n], -1.0, LBM1[:, h, d:d + 1], ADD, MUL, [k("SG"), "LBM1"], [k("KK")])
                A(SG[d][:, 0:n], SG[d][:, 0:n], AF.Ln, [k("SG"), "OML", "LBT"], [k("SG")], scale=OML[:, h, d:d + 1], bias=LBT[:, h, d:d + 1])
                yield
                p.op("dve", (lambda: lambda e: e.tensor_tensor_scan(out=CIN[d][:, 0:n], data0=ONES32[:, 0:n], data1=SG[d][:, 0:n],
                                                                     initial=0.0, op0=MUL, op1=ADD))(), ["ONES32", k("SG")], [k("CIN")])
                yield
                if d == 1:
                    CP("dve", TOT[d][:], CIN[d][:, n - 1:n], [k("CIN")], [k("TOT")])
                    STT("dve", D1[d][:, 0:n], CIN[d][:, 0:n], -1.0, SG[d][:, 0:n], MUL, ADD, [k("CIN"), k("SG")], [k("D1")])
                    TS("dve", CIN[d][:, 0:n], D1[d][:, 0:n], TOT[d][:, 0:1], None, ADD, None, [k("D1"), k("TOT")], [k("CIN")])
                    yield
                TT("dve", PIN[d][:, 0:nch], cv(CIN[d])[:, :, a_i], cv(SG[d])[:, :, a_i], SUB, [k("CIN"), k("SG")], [k("PIN")])
                TT("dve", DCH[d][:, 0:nch], cv(CIN[d])[:, :, z_i], PIN[d][:, 0:nch], SUB, [k("CIN"), k("PIN")], [k("DCH")])
                A(DCH[d][:, 0:nch], DCH[d][:, 0:nch], AF.Exp, [k("DCH")], [k("DCH")])
                yield
                TT("dve", cv(D1[d]), cv(CIN[d]), bc(cv(CIN[d])[:, :, z_i]), SUB, [k("CIN")], [k("D1")])
                yield
                A(D1[d][:, 0:n], D1[d][:, 0:n], AF.Exp, [k("D1")], [k("D1")], scale=-1.0)
                yield
                TT("dve", KBT[d][:, 0:n], KK[d][:, 0:n], D1[d][:, 0:n], MUL, [k("KK"), k("D1")], [k("KBT")])
                yield
                TR([(PBH[0:64, kk_ * 128:(kk_ + 1) * 128], KBT[d][:, kk_ * 64:(kk_ + 1) * 64]) for kk_ in range(nch)], IDENT[:],
                   [k("KBT"), "IDENT"], [PBHk])
                yield
                CP("act", KBCH[d][0:64, 0:nch, :], PBH[0:64, 0:nch * 128].rearrange("p (c e) -> p c e", c=nch), [PBHk], [k("KBCH")])
                yield
                MM([(PB[2 + d][:, kk_ * 128:(kk_ + 1) * 128], KBCH[d][0:64, kk_, :], VCH[0:64, ch0 + kk_, :], True, True)
                    for kk_ in range(nch)], [k("KBCH")] + tkeys("VCH", t0, t1), [PBk[2 + d]])
                yield
                if not is_ctx:
                    TT("dve", cv(D1[d]), cv(CIN[d]), bc(PIN[d][:, 0:nch]), SUB, [k("CIN"), k("PIN")], [k("D1")])
                    yield
                    A(D1[d][:, 0:n], D1[d][:, 0:n], AF.Exp, [k("D1")], [k("D1")])
                    yield
                    TT("dve", QB[d][:, 0:n], QS[:, t0:t1], D1[d][:, 0:n], MUL, tkeys("QS", t0, t1) + [k("D1")], [k("QB")])
                    TT("dve", cv(D2[d]), cv(CIN[d]), bc(cv(CIN[d])[:, :, m_i]), SUB, [k("CIN")], [k("D2")])
                    yield
                    A(SG[d][:, 0:n], D2[d][:, 0:n], AF.Exp, [k("D2")], [k("SG")], scale=-1.0)
                    A(D2[d][:, 0:n], D2[d][:, 0:n], AF.Exp, [k("D2")], [k("D2")])
                    yield
                    TT("dve", QTL[d][:, 0:n], QS[:, t0:t1], D2[d][:, 0:n], MUL, tkeys("QS", t0, t1) + [k("D2")], [k("QTL")])
                    TT("dve", KTL[d][:, 0:n], KK[d][:, 0:n], SG[d][:, 0:n], MUL, [k("KK"), k("SG")], [k("KTL")])
                    yield
                    mms = []
                    for kk_ in range(nch):
                        c0 = kk_ * 64
                        if d == 0:
                            mms.append((PB[4][0:32, c0:c0 + 64], KTL[d][:, c0:c0 + 32], QTL[d][:, c0:c0 + 64], True, True))
                            mms.append((PB[4][32:64, c0 + 32:c0 + 64], KTL[d][:, c0 + 32:c0 + 64], QTL[d][:, c0 + 32:c0 + 64], True, True))
                        else:
                            mms.append((PB[5][0:32, c0:c0 + 32], KTL[d][:, c0:c0 + 32], QTL[d][:, c0:c0 + 32], True, True))
                            mms.append((PB[5][32:64, c0:c0 + 64], KTL[d][:, c0 + 32:c0 + 64], QTL[d][:, c0:c0 + 64], True, True))
                    MM(mms, [k("KTL"), k("QTL")], [PBk[4 + d]])
                    yield
                    TT("dve", AT[d][0:64, 0:n].rearrange("p (c j) -> p c j", j=64), PB[4 + d][0:64, 0:n].rearrange("p (c j) -> p c j", j=64),
                       MASK[d][:].unsqueeze(1).to_broadcast([64, nch, 64]), MUL, [PBk[4 + d], "MASK"], [k("AT")])
                    yield
                for kk_ in (range(nch) if d == 0 else range(nch - 1, -1, -1)):
                    if not is_ctx:
                        CP("act", SBF[d][:, kk_, :], S[d][:], [k("S")], [("SBF", d, kk_)])
                    STT("dve", S[d][:], S[d][:], DCH[d][:, kk_:kk_ + 1], PB[2 + d][:, kk_ * 128:(kk_ + 1) * 128], MUL, ADD,
                        [k("S"), k("DCH"), PBk[2 + d]], [k("S")])
                    yield
                if is_ctx:
                    return
                po = PB[4 + d][:, BL:BL + n]
                pok = PBk[4 + d]
                mms = []
                for kk_ in range(nch):
                    c0 = kk_ * 64
                    mms.append((PB[4 + d][:, BL + c0:BL + c0 + 64], SBF[d][:, kk_, :], QB[d][:, c0:c0 + 64], True, False))
                    mms.append((PB[4 + d][:, BL + c0:BL + c0 + 64], VCH[0:64, ch0 + kk_, :], AT[d][0:64, c0:c0 + 64], False, True))
                MM(mms, [("SBF", d, kk_) for kk_ in range(nch)] + [k("QB"), k("AT")] + tkeys("VCH", t0, t1), [pok])
                yield
                ok = [("OT", t0 // BL)]
                if first_writer:
                    CP("act", OT[:, t0:t1], po, [pok], ok)
                    yield
                    return
                TT("dve", OT[:, t0:t1], OT[:, t0:t1], po, ADD, [pok] + ok, ok)
                yield
                pg, pk = proj(i, 4, t0, t1)
                yield
                sigm(SGG[d][:, 0:n], pg, [pk], [k("SGG")])
                TT("dve", SGG[d][:, 0:n], SGG[d][:, 0:n], pg, MUL, [k("SGG"), pk], [k("SGG")])
                yield
                A(SQ[d][:, 0:n], OT[:, t0:t1], AF.Square, ok, [k("HSQ")])
                yield
                j_ = PJB[cnt["pj"] % 3]
                cnt["pj"] += 1
                pss, pssk = PB[j_][:, 0:n], PBk[j_]
                MM([(pss, ONESB[:], SQ[d][:, 0:n], True, True)], ["ONESB", k("HSQ")], [pssk])
                yield
                A(RS[d][:, 0:n], pss, AF.Ln, [pssk, "EPSC"], [k("HRS")], scale=1.0 / 128, bias=EPSC[:, 0:1])
                A(RS[d][:, 0:n], RS[d][:, 0:n], AF.Exp, [k("HRS")], [k("HRS")], scale=-0.5)
                yield
                STT("dve", D2[d][:, 0:n], OT[:, t0:t1], GOUT[:, 0:1], RS[d][:, 0:n], MUL, MUL, ok + ["GOUT", k("HRS")], [k("D2")])
                yield
                TT("dve", ONT[i][:, t0:t1], D2[d][:, 0:n], SGG[d][:, 0:n], MUL, [k("D2"), k("SGG")], tkeys(("HONT", i), t0, t1))
                yield

            def run_interleaved(gens):
                gens = list(gens)
                while gens:
                    for g in list(gens):
                        try:
                            next(g)
                        except StopIteration:
                            gens.remove(g)

            def gen_wo(hh):
                ii = hh % 2
                for t in range(16):
                    for dbl in range(2):
                        b_ = PJB[cnt["pj"] % 3]
                        cnt["pj"] += 1
                        ds = slice(dbl * 512, (dbl + 1) * 512)
                        MM([(PB[b_][:], ONT[ii][:, t * 128:(t + 1) * 128], WOG[ii][:, ds], True, True)], [(("HONT", ii), t), ("HWOG", ii)], [PBk[b_]])
                        TT("dve", X[:, t, ds], X[:, t, ds], PB[b_][:], ADD, [("X", t), PBk[b_]], [("X", t)])
                        yield

            for h in range(8):
                i = h % 2
                if h + 1 < 8:
                    load(h + 1)
                TT("pool", WOG[i][:], WO[i][:], G_l[:], MUL, [("HWO", i), "G_l"], [("HWOG", i)])
                blks = [ctx_block] + lat_blocks

                def v_tail(idx):
                    (t0, t1) = blks[idx]
                    n = t1 - t0
                    nch = n // 64
                    ch0 = t0 // 64
                    vb = idx % 2
                    TR([(PBH[0:64, kk_ * 128:(kk_ + 1) * 128], VTF[vb][:, kk_ * 64:(kk_ + 1) * 64]) for kk_ in range(nch)], IDENT[:],
                       [("VTF", vb), "IDENT"], [PBHk])
                    CP("dve", VCH[0:64, ch0:ch0 + nch, :], PBH[0:64, 0:nch * 128].rearrange("p (c e) -> p c e", c=nch), [PBHk],
                       tkeys("VCH", t0, t1))
                for idx, (t0, t1) in enumerate(blks):
                    n = t1 - t0
                    pq, pk = proj(i, 0, t0, t1)
                    A(QS[:, t0:t1], pq, AF.Silu, [pk], tkeys("QS", t0, t1))
                    pv, pk = proj(i, 1, t0, t1)
                    CP("dve", VTF[idx % 2][:, 0:n], pv, [pk], [("VTF", idx % 2)])
                    if idx >= 1:
                        v_tail(idx - 1)
                v_tail(len(blks) - 1)
                for d in range(2):
                    MEMSET("dve", S[d][:], 0.0, [("S", d)])
                bg = [gen_wo(h - 1)] if h > 0 else []
                for s_ in range(NB + 1):
                    if s_ == 0:
                        gens = [chain(h, i, 0, ctx_block, False), chain(h, i, 1, ctx_block, False)]
                    else:
                        fw = s_ <= NB // 2
                        gens = [chain(h, i, 0, lat_blocks[s_ - 1], fw), chain(h, i, 1, lat_blocks[NB - s_], fw)]
                    run_interleaved(gens + bg)
            run_interleaved([gen_wo(7)])
            p.barrier()

    out_streamed = [False]

    def finish():
        yv = yout.rearrange("(n p) d -> p n d", p=128)
        if not out_streamed[0]:
            for g in range(0, 16, 4):
                DMA("sp", "yout", [(yv[:, g:g + 4, :], X[:, g:g + 4, :])], [("X", t) for t in range(g, g + 4)], [])
        slots = ["yout"] + (["tap"] if B.tap_outs else [])
        p.emit(final_wait_slots=slots)
        return B

    norm_phase(0, "mix", NT)
    B.tap("HT0", HT[:], [128, 8, T], [("HT", t) for t in range(NT)], BF16)
    B.tap("G_l0", G_l[:], [128, D], ["G_l"])
    if stop == "norm0":
        return finish()
    attention_phase()
    B.tap("XMIX0", X[:], [128, NT, D], [("X", t) for t in range(NT)])
    if stop == "attn0":
        return finish()
    norm_phase(0, "ffn", NT)
    moe_phase(0, NT)
    B.tap("XFFN0", X[:], [128, NT, D], [("X", t) for t in range(NT)])
    if stop == "moe0":
        return finish()
    norm_phase(1, "mix", NT)
    hgrn_phase()
    B.tap("XMIX1", X[:], [128, NT, D], [("X", t) for t in range(NT)])
    if stop == "hgrn1":
        return finish()
    norm_phase(1, "ffn", 16)
    moe_phase(1, 16)
    return finish()


def _consts():
    ident = np.eye(128, dtype=np.float32)
    blk = np.zeros((128, 128), np.float32)
    blk[:64, :64] = 1.0
    blk[64:, 64:] = 1.0
    rm = np.zeros((128, 128), np.float32)
    for fp in range(128):
        j = fp % 64
        if j < 32:
            rm[fp + 32, fp] = -1.0
        else:
            rm[fp - 32, fp] = 1.0
    t = np.arange(NLAT)
    r = (t // 64).astype(np.float32)
    col = (t % 64).astype(np.float32)
    inv = (10000.0 ** (-np.arange(16, dtype=np.float32) / 16)).astype(np.float32)
    ang = np.concatenate([r[:, None] * inv, col[:, None] * inv], axis=-1).astype(np.float32)
    cosT = np.cos(ang).astype(np.float32).T
    sinT = np.sin(ang).astype(np.float32).T
    cos = np.tile(cosT, (4, 1))
    sin = np.tile(sinT, (4, 1))
    s_idx = np.arange(64)[:, None]
    t_idx = np.arange(64)[None, :]
    maskf = (s_idx <= t_idx).astype(np.float32)
    maskb = (s_idx >= t_idx).astype(np.float32)
    return dict(cst_ident=ident, cst_blk=blk, cst_rm=rm, cst_cos=np.ascontiguousarray(cos), cst_sin=np.ascontiguousarray(sin),
                cst_maskf=maskf, cst_maskb=maskb)


def make_in_maps(inputs):
    f = lambda a: np.ascontiguousarray(np.asarray(a, dtype=np.float32))
    shared = dict(
        ada_w=f(inputs["ada_w"]), ada_b=f(inputs["ada_b"]), norm_mix=f(inputs["norm_mix"]), norm_ffn=f(inputs["norm_ffn"]),
        attn_w_qkv=f(inputs["attn_w_qkv"])[0], attn_w_o=f(inputs["attn_w_o"])[0], attn_q_norm=f(inputs["attn_q_norm"])[0],
        attn_k_norm=f(inputs["attn_k_norm"])[0], attn_sub_norm=f(inputs["attn_sub_norm"])[0],
        attn_lambda=f(inputs["attn_lambda"])[0].reshape(256), hgrn_w_in=f(inputs["hgrn_w_in"])[0],
        hgrn_w_o=f(inputs["hgrn_w_o"])[0], hgrn_out_norm=f(inputs["hgrn_out_norm"])[0],
        hgrn_lb_gamma=f(inputs["hgrn_lb_gamma"]), router_w=f(inputs["router_w"]), router_bias=f(inputs["router_bias"]),
        moe_w_gate=f(inputs["moe_w_gate"]), moe_w_up=f(inputs["moe_w_up"]), moe_w_down=f(inputs["moe_w_down"]),
    )
    shared.update(_consts())
    x = f(inputs["x"])
    ctx = f(inputs["ctx"])
    c = f(inputs["c"])
    c_ctx = f(inputs["c_ctx"])
    maps = []
    for b in range(8):
        m = dict(shared)
        m["xin"] = np.ascontiguousarray(np.concatenate([x[b], ctx[b]], axis=0))
        m["cc"] = np.ascontiguousarray(np.stack([c[b], c_ctx], axis=0))
        maps.append(m)
    return maps


def kernel(**inputs):
    B = build()
    maps = make_in_maps(inputs)
    res = run_bass_kernel_spmd(B.nc, maps, core_ids=list(range(8)))
    return np.stack([np.asarray(r["y"], dtype=np.float32) for r in res.results], axis=0)
```
